# Optimizing a Trainium2 kernel written in Bass

```python
import jax, jax.numpy as jnp
from jax import lax
import numpy as np

D_MODEL = 1024
BATCH = 8
SEQ = 2048
DEPTH = 1

GRID_W = 64
CTX_LEN = 256
MIX_WIDTH = D_MODEL
HGRN_WIDTH = MIX_WIDTH // 2
HGRN_HEAD_DIM = 128
HGRN_HEADS = HGRN_WIDTH // HGRN_HEAD_DIM
FNET_WIDTH = MIX_WIDTH - HGRN_WIDTH
FNET_GROUPS = 4
FNET_GROUP_DIM = FNET_WIDTH // FNET_GROUPS
IN_PROJ_WIDTH = 5 * HGRN_WIDTH + FNET_WIDTH
CHUNK = 64
N_EXPERTS = 32
TOP_K = 4
D_FF = D_MODEL
SWIGLU_LIMIT = 7.0
SWIGLU_ALPHA = 1.702
NORM_EPS = 1e-6
POS_BASE = 10000.0

kernel_name = "hybrid_hgrn2_fnet_moe_dit"


def _rmsnorm(x, g):
    xf = x.astype(jnp.float32)
    y = xf * lax.rsqrt(jnp.mean(xf * xf, axis=-1, keepdims=True) + NORM_EPS)
    return (y * g.astype(jnp.float32)).astype(x.dtype)


def _modulate(h, shift, scale):
    return h * (1 + scale) + shift


def _sincos_2d(rows, cols, dim):
    quarter = dim // 4
    omega = 1.0 / (POS_BASE ** (jnp.arange(quarter, dtype=jnp.float32) / quarter))

    def axis_emb(n):
        ang = jnp.arange(n, dtype=jnp.float32)[:, None] * omega[None, :]
        return jnp.concatenate([jnp.sin(ang), jnp.cos(ang)], axis=-1)

    er = jnp.broadcast_to(axis_emb(rows)[:, None, :], (rows, cols, dim // 2))
    ec = jnp.broadcast_to(axis_emb(cols)[None, :, :], (rows, cols, dim // 2))
    return jnp.concatenate([er, ec], axis=-1).reshape(rows * cols, dim)


def _heads(t):
    b, l, _ = t.shape
    return t.reshape(b, l, HGRN_HEADS, HGRN_HEAD_DIM).transpose(0, 2, 1, 3)


def _gla_chunk(q, k, v, logf, s0):
    b_, h_, l_, dk = q.shape
    dv = v.shape[-1]
    n = l_ // CHUNK
    r = lambda t: t.reshape(b_, h_, n, CHUNK, t.shape[-1])
    q, k, v, g = r(q), r(k), r(v), r(logf)
    bcum = jnp.cumsum(g, axis=3)
    b_ref = bcum[:, :, :, CHUNK // 2 - 1:CHUNK // 2, :]
    qi = q * jnp.exp(bcum - b_ref)
    ki = k * jnp.exp(b_ref - bcum)
    a = jnp.einsum('bhntd,bhnsd->bhnts', qi, ki)
    a = jnp.where(jnp.tril(jnp.ones((CHUNK, CHUNK), dtype=bool)), a, 0.0)
    o_intra = jnp.einsum('bhnts,bhnsv->bhntv', a, v)
    b_last = bcum[:, :, :, -1:, :]
    u = jnp.einsum('bhnsd,bhnsv->bhndv', k * jnp.exp(b_last - bcum), v)
    decay = jnp.exp(b_last[:, :, :, 0, :])

    def step(s, xs):
        d, uu = xs
        return d[..., None] * s + uu, s

    s_final, s_start = lax.scan(step, s0, (jnp.moveaxis(decay, 2, 0), jnp.moveaxis(u, 2, 0)))
    s_start = jnp.moveaxis(s_start, 0, 2)
    o_inter = jnp.einsum('bhntd,bhndv->bhntv', q * jnp.exp(bcum), s_start)
    return (o_intra + o_inter).reshape(b_, h_, l_, dv), s_final


def _gla_dir(q, k, v, logf, s0, reverse):
    if reverse:
        q, k, v, logf = (jnp.flip(t, axis=2) for t in (q, k, v, logf))
    o, s = _gla_chunk(q, k, v, logf, s0)
    if reverse:
        o = jnp.flip(o, axis=2)
    return o, s


def _hgrn_inputs(p, lb):
    w = HGRN_WIDTH
    q = _heads(p[..., :w]).astype(jnp.float32)
    v = _heads(p[..., w:2 * w]).astype(jnp.float32)
    g = p[..., 2 * w:3 * w]
    dirs = []
    for d in range(2):
        z = _heads(p[..., (3 + d) * w:(4 + d) * w]).astype(jnp.float32)
        lbd = lb[d].reshape(HGRN_HEADS, 1, HGRN_HEAD_DIM)
        logf = jnp.log(lbd + (1 - lbd) * jax.nn.sigmoid(z))
        k = (1 - lbd) * jax.nn.sigmoid(-z)
        dirs.append((k, logf))
    return q, v, g, dirs


def _hgrn_out(o, g, gn):
    o = o * lax.rsqrt(jnp.mean(o * o, axis=-1, keepdims=True) + NORM_EPS) * gn.astype(jnp.float32)
    b_, h_, l_, dv = o.shape
    o = o.transpose(0, 2, 1, 3).reshape(b_, l_, h_ * dv)
    return (o * jax.nn.sigmoid(g.astype(jnp.float32))).astype(g.dtype)


def _hgrn_mixer(p_lat, p_ctx, lb, gn):
    ql, vl, gl, dl = _hgrn_inputs(p_lat, lb)
    qc, vc, gc, dc = _hgrn_inputs(p_ctx, lb)
    b_ = p_lat.shape[0]
    s0 = jnp.zeros((b_, HGRN_HEADS, HGRN_HEAD_DIM, HGRN_HEAD_DIM), jnp.float32)
    o_lat = 0.0
    o_ctx = 0.0
    for d, rev in enumerate((False, True)):
        oc, sc = _gla_dir(qc, dc[d][0], vc, dc[d][1], s0, rev)
        ol, _ = _gla_dir(ql, dl[d][0], vl, dl[d][1], sc, rev)
        o_lat = o_lat + ol
        o_ctx = o_ctx + oc
    return _hgrn_out(o_lat, gl, gn), _hgrn_out(o_ctx, gc, gn)


def _fourier_mix(u, w):
    b_, l_, _ = u.shape
    ug = u.reshape(b_, l_, FNET_GROUPS, FNET_GROUP_DIM).astype(jnp.float32)
    f = jnp.fft.fft2(ug, axes=(1, 3), norm='ortho').real
    y = jnp.einsum('blgc,gcd->blgd', f, w.astype(jnp.float32))
    return y.reshape(b_, l_, FNET_WIDTH).astype(u.dtype)


def _moe(h, w_r, b_r, w1, b1, w2, b2):
    b_, l_, d = h.shape
    t = h.reshape(b_ * l_, d)
    logits = (t @ w_r + b_r).astype(jnp.float32)
    top_v, top_i = lax.top_k(logits, TOP_K)
    wts = jax.nn.softmax(top_v, axis=-1)
    gate = jnp.sum(jax.nn.one_hot(top_i, N_EXPERTS, dtype=jnp.float32) * wts[..., None], axis=1)
    gate = gate.astype(t.dtype)
    out = jnp.zeros_like(t)
    for e in range(N_EXPERTS):
        a = t @ w1[e] + b1[e]
        a_glu = jnp.minimum(a[:, :D_FF], SWIGLU_LIMIT)
        a_lin = jnp.clip(a[:, D_FF:], -SWIGLU_LIMIT, SWIGLU_LIMIT)
        hh = a_glu * jax.nn.sigmoid(SWIGLU_ALPHA * a_glu) * (a_lin + 1)
        out = out + gate[:, e:e + 1] * (hh @ w2[e] + b2[e])
    return out.reshape(b_, l_, d)


def _normal(k, shape, s):
    return jax.random.normal(k, shape, jnp.float32) * s


def setup_inputs(seed: int = 0) -> dict:
    key = jax.random.key(seed)
    ks = jax.random.split(key, 20)
    d = D_MODEL
    return {
        "x": _normal(ks[0], (BATCH, SEQ, d), 1.0),
        "c": _normal(ks[1], (BATCH, d), 1.0),
        "ctx": _normal(ks[2], (BATCH, CTX_LEN, d), 1.0),
        "c_ctx": _normal(ks[3], (d,), 1.0),
        "ada_w": _normal(ks[4], (DEPTH, d, 6 * d), d ** -0.5),
        "ada_b": _normal(ks[5], (DEPTH, 6 * d), 0.02),
        "norm1_g": 1.0 + _normal(ks[6], (DEPTH, d), 0.05),
        "w_in": _normal(ks[7], (DEPTH, d, IN_PROJ_WIDTH), d ** -0.5),
        "hgrn_lb": _normal(ks[8], (DEPTH + 1, 2, HGRN_WIDTH), 0.2),
        "hgrn_gnorm": 1.0 + _normal(ks[9], (DEPTH, HGRN_HEAD_DIM), 0.05),
        "fnet_w": _normal(ks[10], (DEPTH, FNET_GROUPS, FNET_GROUP_DIM, FNET_GROUP_DIM), FNET_GROUP_DIM ** -0.5),
        "w_out": _normal(ks[11], (DEPTH, MIX_WIDTH, d), MIX_WIDTH ** -0.5),
        "norm2_g": 1.0 + _normal(ks[12], (DEPTH, d), 0.05),
        "router_w": _normal(ks[13], (DEPTH, d, N_EXPERTS), d ** -0.5),
        "router_b": _normal(ks[14], (DEPTH, N_EXPERTS), 0.01),
        "moe_w1": _normal(ks[15], (DEPTH, N_EXPERTS, d, 2 * D_FF), d ** -0.5),
        "moe_b1": _normal(ks[16], (DEPTH, N_EXPERTS, 2 * D_FF), 0.02),
        "moe_w2": _normal(ks[17], (DEPTH, N_EXPERTS, D_FF, d), D_FF ** -0.5),
        "moe_b2": _normal(ks[18], (DEPTH, N_EXPERTS, d), 0.02),
        "final_g": 1.0 + _normal(ks[19], (d,), 0.05),
    }


def reference(x, c, ctx, c_ctx, ada_w, ada_b, norm1_g, w_in, hgrn_lb, hgrn_gnorm, fnet_w, w_out,
              norm2_g, router_w, router_b, moe_w1, moe_b1, moe_w2, moe_b2, final_g):
    n_lat = x.shape[1]
    rows = n_lat // GRID_W
    x = x + _sincos_2d(rows, GRID_W, D_MODEL).astype(x.dtype)[None]
    lb_all = jnp.cumsum(jax.nn.softmax(hgrn_lb.astype(jnp.float32), axis=0), axis=0)
    w = HGRN_WIDTH
    xc = ctx
    for l in range(DEPTH):
        last = l == DEPTH - 1
        mod_x = jax.nn.silu(c) @ ada_w[l] + ada_b[l]
        mod_c = jax.nn.silu(c_ctx) @ ada_w[l] + ada_b[l]
        mx = jnp.split(mod_x[:, None, :], 6, axis=-1)
        mc = jnp.split(mod_c, 6, axis=-1)
        px = _modulate(_rmsnorm(x, norm1_g[l]), mx[0], mx[1]) @ w_in[l]
        pc = _modulate(_rmsnorm(xc, norm1_g[l]), mc[0], mc[1]) @ w_in[l]
        hx, hc = _hgrn_mixer(px[..., :5 * w], pc[..., :5 * w], lb_all[l], hgrn_gnorm[l])
        fx = _fourier_mix(px[..., 5 * w:], fnet_w[l])
        x = x + mx[2] * (jnp.concatenate([hx, fx], axis=-1) @ w_out[l])
        hm = _modulate(_rmsnorm(x, norm2_g[l]), mx[3], mx[4])
        x = x + mx[5] * _moe(hm, router_w[l], router_b[l], moe_w1[l], moe_b1[l], moe_w2[l], moe_b2[l])
        if not last:
            fc = _fourier_mix(pc[..., 5 * w:], fnet_w[l])
            xc = xc + mc[2] * (jnp.concatenate([hc, fc], axis=-1) @ w_out[l])
            hmc = _modulate(_rmsnorm(xc, norm2_g[l]), mc[3], mc[4])
            xc = xc + mc[5] * _moe(hmc, router_w[l], router_b[l], moe_w1[l], moe_b1[l], moe_w2[l], moe_b2[l])
    return _rmsnorm(x, final_g)
```

```python
import numpy as np
import ml_dtypes
from contextlib import ExitStack
import concourse.bass as bass
import concourse.mybir as mybir
from concourse.bass_utils import run_bass_kernel_spmd

F32 = mybir.dt.float32
BF16 = mybir.dt.bfloat16
AF = mybir.ActivationFunctionType
ALU = mybir.AluOpType
AX = mybir.AxisListType

D = 1024
L = 2048
LC = 256
NT = 16
NTA = 18
NE = 32
EPS = 1e-6

STREAM_OF = {"pe": "pe", "act": "act", "dve": "dve", "pool": "pool",
             "sp": "sp", "poolq": "pool", "actq": "act"}


class Op:
    __slots__ = ("eng", "fn", "is_dma", "semkey", "phase", "seq", "needs_inc",
                 "inc_val", "waits", "dma_val", "stream")


class Prog:
    def __init__(self, nc, sem_alloc):
        self.nc = nc
        self.sem_alloc = sem_alloc
        self.phase = 0
        self.ops = []
        self.last_w = {}
        self.readers = {}
        self.eng_sem = {}
        self.eng_cnt = {s: 0 for s in ("pe", "act", "dve", "pool", "sp")}
        self.dma_sem = {}
        self.dma_cnt = {}
        self.waited = {}
        self.seqc = {s: 0 for s in ("pe", "act", "dve", "pool", "sp")}
        self.out_keys = []
        self.nops = 0

    def _sem_for_stream(self, s):
        if s not in self.eng_sem:
            self.eng_sem[s] = self.sem_alloc("e_" + s)
        return self.eng_sem[s]

    def _sem_for_key(self, k):
        if k not in self.dma_sem:
            self.dma_sem[k] = self.sem_alloc("d_" + str(k).replace(":", "_"))
            self.dma_cnt[k] = 0
        return self.dma_sem[k]

    def op(self, eng, fn, reads=(), writes=(), semkey=None):
        o = Op()
        o.eng = eng
        o.fn = fn
        o.is_dma = semkey is not None
        o.semkey = semkey
        o.phase = self.phase
        o.stream = STREAM_OF[eng]
        self.seqc[o.stream] += 1
        o.seq = self.seqc[o.stream]
        o.needs_inc = False
        o.inc_val = None
        o.waits = []
        o.dma_val = None
        if o.is_dma:
            self._sem_for_key(semkey)
            self.dma_cnt[semkey] += 16
            o.dma_val = self.dma_cnt[semkey]
        deps = []
        for k in reads:
            w = self.last_w.get(k)
            if w is not None:
                deps.append((w, True))
        for k in writes:
            w = self.last_w.get(k)
            if w is not None:
                deps.append((w, False))
            deps.extend((r, False) for r in self.readers.get(k, ()))
        best = {}
        seen_dma = set()
        for d, raw in deps:
            if d is o:
                continue
            if d.is_dma:
                if id(d) not in seen_dma:
                    seen_dma.add(id(d))
                    o.waits.append(("dma", d))
            else:
                if d.phase != self.phase:
                    continue
                if d.stream == o.stream and not o.is_dma:
                    if not raw or o.stream == "pe":
                        continue
                b = best.get(d.stream)
                if b is None or d.seq > b.seq:
                    best[d.stream] = d
        for s, d in best.items():
            o.waits.append(("eng", d))
        for k in reads:
            self.readers.setdefault(k, []).append(o)
        for k in writes:
            self.last_w[k] = o
            self.readers[k] = []
        self.ops.append(o)
        self.nops += 1
        return o

    def emit(self, final_wait_out=False):
        nc = self.nc
        ops = self.ops
        for o in ops:
            for kind, d in o.waits:
                if kind == "eng":
                    d.needs_inc = True
        for o in ops:
            if not o.is_dma and o.needs_inc:
                self._sem_for_stream(o.stream)
                self.eng_cnt[o.stream] += 1
                o.inc_val = self.eng_cnt[o.stream]
        by_stream = {s: [] for s in ("pe", "act", "dve", "pool", "sp")}
        for o in ops:
            by_stream[o.stream].append(o)
        waited = self.waited
        prog = self

        def run(stream, e):
            for o in by_stream[stream]:
                for kind, d in o.waits:
                    if kind == "dma":
                        sem = prog.dma_sem[d.semkey]
                        nm = ("d", d.semkey)
                        if str(d.semkey).startswith("G:"):
                            val = prog.dma_cnt[d.semkey]
                        else:
                            val = d.dma_val
                    else:
                        sem = prog.eng_sem[d.stream]
                        nm = ("e", d.stream)
                        val = d.inc_val
                    if waited.get((stream, nm), 0) >= val:
                        continue
                    waited[(stream, nm)] = val
                    e.wait_ge(sem, val)
                ins = o.fn(e)
                if o.is_dma:
                    ins.then_inc(prog.dma_sem[o.semkey], 16)
                elif o.needs_inc:
                    ins.then_inc(prog.eng_sem[o.stream], 1)
            if final_wait_out and stream == "sp":
                for k in prog.out_keys:
                    e.wait_ge(prog.dma_sem[k], prog.dma_cnt[k])

        with nc.Block() as block:
            if by_stream["sp"] or final_wait_out:
                @block.sync
                def _(e):
                    run("sp", e)
            if by_stream["pe"]:
                @block.tensor
                def _(e):
                    run("pe", e)
            if by_stream["act"]:
                @block.scalar
                def _(e):
                    run("act", e)
            if by_stream["dve"]:
                @block.vector
                def _(e):
                    run("dve", e)
            if by_stream["pool"]:
                @block.gpsimd
                def _(e):
                    run("pool", e)
        self.ops = []
        self.phase += 1


def _pos_table():
    quarter = D // 4
    omega = (1.0 / (np.float32(10000.0) ** (np.arange(quarter, dtype=np.float32) / np.float32(quarter)))).astype(np.float32)

    def axis_emb(n):
        ang = np.arange(n, dtype=np.float32)[:, None] * omega[None, :]
        return np.concatenate([np.sin(ang), np.cos(ang)], axis=-1).astype(np.float32)

    rows, cols = L // 64, 64
    er = np.broadcast_to(axis_emb(rows)[:, None, :], (rows, cols, D // 2))
    ec = np.broadcast_to(axis_emb(cols)[None, :, :], (rows, cols, D // 2))
    return np.ascontiguousarray(np.concatenate([er, ec], axis=-1).reshape(rows * cols, D)).astype(np.float32)


def _consts():
    c = {}
    c["pos"] = _pos_table()
    c["ident"] = np.eye(128, dtype=np.float32)
    c["ones"] = np.ones((128, 128), dtype=np.float32)
    s = np.arange(128)
    same = (s[:, None] // 64) == (s[None, :] // 64)
    si = (s % 64)[:, None]
    ti = (s % 64)[None, :]
    mi0 = same & (si <= ti)
    mc0 = same * ((si <= ti).astype(np.float32) - (si <= 31).astype(np.float32))
    m20 = same & (si > ti)
    mi1 = same & (si >= ti)
    mc1 = same * ((si >= ti).astype(np.float32) - (si >= 32).astype(np.float32))
    m21 = same & (si < ti)
    sel = np.zeros((128, 2), np.float32)
    sel[:64, 0] = 1
    sel[64:, 1] = 1
    p_in = (s % 64)[:, None]
    t_in = np.arange(64)[None, :]
    mask0 = (p_in <= t_in).astype(np.float32)
    mask1 = (p_in >= t_in).astype(np.float32)
    cm = np.concatenate([mc0, mi0, m20, mc1, mi1, m21, sel, mask0, mask1], axis=1).astype(np.float32)
    c["cm"] = np.ascontiguousarray(cm)
    k = np.arange(128, dtype=np.float64)
    ang = 2.0 * np.pi * np.outer(k, k) / 128.0
    c["dftc"] = np.ascontiguousarray(np.concatenate([np.cos(ang) / 512.0, -np.sin(ang) / 512.0], axis=1)).astype(np.float32)
    kk = np.arange(L, dtype=np.int64)
    ph = (np.outer(kk, kk) % L).astype(np.float64) * (2.0 * np.pi / L)
    mats = [np.cos(ph), np.sin(ph)]
    pieces = np.empty((4, 2, 2, 128, 8, 512), dtype=ml_dtypes.bfloat16)
    for cs in range(2):
        m = mats[cs].reshape(2, 8, 128, 4, 512)
        pieces[:, cs] = np.transpose(m, (3, 0, 2, 1, 4)).astype(ml_dtypes.bfloat16)
    c["dftl"] = np.ascontiguousarray(pieces.reshape(16, 128, 4096))
    return c


_CONSTS = None


def _get_consts():
    global _CONSTS
    if _CONSTS is None:
        _CONSTS = _consts()
    return _CONSTS


def build(stop_after=None):
    nc = bass.Bass("TRN2", target_bir_lowering=False)

    def din(name, shape, dt=F32):
        return nc.dram_tensor(name, list(shape), dt, kind="ExternalInput").ap()

    x_d = din("x", [L, D])
    pos_d = din("pos", [L, D])
    ctx_d = din("ctx", [LC, D])
    cT_d = din("cT", [128, 16])
    adaw_d = din("ada_w", [D, 6 * D])
    adab_d = din("ada_bT", [128, 48])
    n1g_d = din("n1gT", [128, 8])
    n2g_d = din("n2gT", [128, 8])
    win_d = din("w_in", [D, 3072])
    lbT_d = din("lbT", [128, 16])
    lbrow_d = din("lbrow", [1, 2048])
    gn_d = din("gn", [128, 1])
    fw_d = din("fnet_w", [128, 512])
    wout_d = din("w_out", [D, D])
    rw_d = din("rw", [128, 256])
    rb_d = din("rb", [1, 32])
    w1_d = din("w1r", [NE * 8, 128, 2048])
    b1_d = din("b1T", [128, NE * 16])
    w2_d = din("w2", [NE * 8, 128, D])
    b2_d = din("b2", [NE, D])
    fg_d = din("fg", [1, D])
    ident_d = din("ident", [128, 128])
    ones_d = din("ones", [128, 128])
    cm_d = din("cm", [128, 898])
    dftc_d = din("dftc", [128, 256])
    dftl_d = din("dftl", [16, 128, 4096], BF16)
    out_d = nc.dram_tensor("out", [L, D], F32, kind="ExternalOutput").ap()
    dbg_d = nc.dram_tensor("dbg", [128, 4608], F32, kind="ExternalOutput").ap() if stop_after is not None else None

    with ExitStack() as es:
        def sb(name, shape, dt=F32, stack=es):
            return stack.enter_context(nc.sbuf_tensor(name, list(shape), dt))

        def sem_alloc(name):
            return es.enter_context(nc.semaphore(name))

        P = Prog(nc, sem_alloc)
        PS = [es.enter_context(nc.psum_tensor("ps%d" % i, [128, 512], F32)) for i in range(8)]
        psi = [0]

        def nps():
            i = psi[0] % 8
            psi[0] += 1
            return PS[i], "ps%d" % i

        def dma(q, out, in_, reads, writes, key):
            P.op(q, lambda e: e.dma_start(out=out, in_=in_), reads, writes, semkey=key)

        def mm(out, lhsT, rhs, st, sp, reads, writes):
            P.op("pe", lambda e: e.matmul(out, lhsT, rhs, start=st, stop=sp), reads, writes)

        def tr(out, in_, reads, writes):
            P.op("pe", lambda e: e.transpose(out, in_, IDENT[:]), list(reads) + ["ident"], writes)

        def act(out, in_, func, reads, writes, bias=None, scale=None, accum=None):
            kw = {}
            if bias is not None:
                kw["bias"] = bias
            if scale is not None:
                kw["scale"] = scale
            if accum is not None:
                kw["accum_out"] = accum
            P.op("act", lambda e: e.activation(out, in_, func, **kw), reads, writes)

        def ts(eng, out, in0, s1, s2, op0, op1, reads, writes):
            if op1 is None:
                P.op(eng, lambda e: e.tensor_scalar(out, in0, s1, None, op0), reads, writes)
            else:
                P.op(eng, lambda e: e.tensor_scalar(out, in0, s1, s2, op0, op1), reads, writes)

        def tt(eng, out, in0, in1, op, reads, writes):
            P.op(eng, lambda e: e.tensor_tensor(out, in0, in1, op), reads, writes)

        def stt(eng, out, in0, scalar, in1, op0, op1, reads, writes):
            P.op(eng, lambda e: e.scalar_tensor_tensor(out, in0, scalar, in1, op0, op1), reads, writes)

        def cp(eng, out, in_, reads, writes):
            P.op(eng, lambda e: e.tensor_copy(out, in_), reads, writes)

        def memset(eng, ap, val, writes):
            P.op(eng, lambda e: e.memset(ap, val), (), writes)

        ACC = sb("ACC", [128, NT * D])
        IDENT = sb("IDENT", [128, 128])
        ONES = sb("ONES", [128, 128])
        GATE = sb("GATE", [128, NT * NE])
        COLS = sb("COLS", [128, 64])
        MODT = sb("MODT", [128, 96])
        MX5BC = sb("MX5BC", [128, D])
        EPSC = sb("EPSC", [128, 1])

        def acck(t):
            return "acc%d" % t

        A1X, A1C, A2C, LBT, OMLT, GNC = 0, 8, 16, 24, 32, 40

        def col(base, i):
            return COLS[:, base + i:base + i + 1]

        def modcol(j, which):
            return MODT[:, j * 2 + which:j * 2 + which + 1]


        with ExitStack() as s1:
            XNT = sb("XNT", [128, 8 * 2304], BF16, stack=s1)
            MX2BC = sb("MX2BC", [128, D], stack=s1)
            CM = sb("CM", [128, 898], stack=s1)
            LBBC = sb("LBBC", [128, 1024], stack=s1)
            OMLBC = sb("OMLBC", [128, 1024], stack=s1)

            def xk(t):
                return "xnt%d" % t

            def xnt(kc, t0, n):
                return XNT[:, kc * 2304 + t0:kc * 2304 + t0 + n]

            with ExitStack() as sa:
                G1 = "G:p1"
                G0 = "G:p0"
                CT = sb("CT", [128, 16], stack=sa)
                SC = sb("SC", [128, 16], stack=sa)
                ADAB = sb("ADAB", [128, 48], stack=sa)
                N1G = sb("N1G", [128, 8], stack=sa)
                N2G = sb("N2G", [128, 8], stack=sa)
                LBTR = sb("LBTR", [128, 16], stack=sa)
                TMPC = sb("TMPC", [128, 16], stack=sa)
                DG = [sb("DG%d" % i, [128, 128], stack=sa) for i in range(2)]
                AW = [sb("AW%d" % i, [128, 8 * 512], stack=sa) for i in range(2)]
                dma("sp", IDENT[:], ident_d, (), ["ident"], G0)
                dma("sp", ONES[:], ones_d, (), ["ones"], G0)
                dma("sp", CT[:], cT_d, (), ["ct"], G0)
                dma("sp", ADAB[:], adab_d, (), ["adab"], G0)
                dma("sp", N1G[:], n1g_d, (), ["n1g"], G0)
                dma("sp", N2G[:], n2g_d, (), ["n2g"], G0)
                dma("sp", LBTR[:], lbT_d, (), ["lbtr"], G0)
                dma("sp", COLS[:, GNC:GNC + 1], gn_d, (), ["gnc"], G0)
                memset("dve", EPSC[:], EPS, ["epsc"])
                act(SC[:], CT[:], AF.Silu, ["ct"], ["sc"])
                adaw_v = adaw_d.rearrange("(kc p) n -> p kc n", p=128)
                pm, pmk = nps()
                for j in range(12):
                    s = j % 2
                    dma("sp", AW[s][:].rearrange("p (kc n) -> p kc n", kc=8), adaw_v[:, :, j * 512:(j + 1) * 512],
                        (), ["aw%d" % s], "aw%d" % s)
                    for oc in range(4):
                        gi = j * 4 + oc
                        for kc in range(8):
                            mm(pm[:, gi * 2:gi * 2 + 2], AW[s][:, kc * 512 + oc * 128:kc * 512 + (oc + 1) * 128],
                               SC[:, kc * 2:kc * 2 + 2], kc == 0, kc == 7, ["aw%d" % s, "sc"], [pmk])
                pm3 = pm[:, 0:96].rearrange("p (c t) -> p c t", t=2)
                md3 = MODT[:, 0:96].rearrange("p (c t) -> p c t", t=2)
                for w in range(2):
                    tt("dve", md3[:, :, w], pm3[:, :, w], ADAB[:], ALU.add, [pmk, "adab"], ["modt"])
                mdx = MODT[:, 0:96].rearrange("p (c t) -> p c t", t=2)
                for (base, j0, w, g, gk) in ((A1X, 8, 0, N1G, "n1g"), (A1C, 8, 1, N1G, "n1g"), (A2C, 32, 0, N2G, "n2g")):
                    ts("dve", TMPC[:, 0:8], mdx[:, j0:j0 + 8, w], 1.0, None, ALU.add, None, ["modt"], ["tmpc"])
                    tt("dve", COLS[:, base:base + 8], TMPC[:, 0:8], g[:], ALU.mult, ["tmpc", gk], ["cols"])
                tt("dve", TMPC[:, 0:8], LBTR[:, 0:8], LBTR[:, 8:16], ALU.subtract, ["lbtr"], ["tmpc"])
                act(COLS[:, LBT:LBT + 8], TMPC[:, 0:8], AF.Sigmoid, ["tmpc"], ["cols"])
                ts("dve", COLS[:, OMLT:OMLT + 8], COLS[:, LBT:LBT + 8], -1.0, 1.0, ALU.mult, ALU.add, ["cols"], ["cols"])
                for c in range(8):
                    if c % 4 == 0:
                        pb, pbk = nps()
                    dg = DG[c % 2]
                    ts("dve", dg[:], IDENT[:], modcol(40 + c, 0), None, ALU.mult, None, ["ident", "modt"], ["dg%d" % (c % 2)])
                    mm(pb[:, (c % 4) * 128:(c % 4 + 1) * 128], ONES[:], dg[:], True, True, ["ones", "dg%d" % (c % 2)], [pbk])
                    if c % 4 == 3:
                        cp("dve", MX5BC[:, (c - 3) * 128:(c + 1) * 128], pb[:], [pbk], ["mx5bc"])
                LBR = sb("LBR", [128, 2048], stack=sa)
                POS = [sb("POS%d" % i, [128, D], stack=sa) for i in range(2)]
                XC = [sb("XC%d" % i, [128, D], stack=sa) for i in range(2)]
                YT = [sb("YT%d" % i, [128, D], stack=sa) for i in range(2)]
                SQJ = sb("SQJ", [128, D], BF16, stack=sa)
                SS = sb("SS", [128, NTA], stack=sa)
                RSTD = sb("RSTD", [128, NTA], stack=sa)
                DG2 = [sb("DGb%d" % i, [128, 128], stack=sa) for i in range(2)]
                dma("sp", CM[:], cm_d, (), ["cm"], G1)
                dma("sp", LBR[:], lbrow_d.partition_broadcast(128), (), ["lbr"], G1)
                def rstd_tile(t):
                    act(RSTD[:, t:t + 1], SS[:, t:t + 1], AF.Sqrt, ["ss%d" % t, "epsc"], ["rstd%d" % t], bias=EPSC[:, 0:1], scale=1.0 / D)
                    P.op("dve", lambda e: e.reciprocal(RSTD[:, t:t + 1], RSTD[:, t:t + 1]), ["rstd%d" % t], ["rstd%d" % t])
                def norm_tile(t):
                    rstd_tile(t)
                    lat = t < NT
                    src = ACC[:, t * D:(t + 1) * D] if lat else XC[t - NT][:]
                    srck = acck(t) if lat else "xc%d" % (t - NT)
                    y = YT[t % 2]
                    yk = "yt%d" % (t % 2)
                    act(y[:], src, AF.Copy, [srck, "rstd%d" % t], [yk], scale=RSTD[:, t:t + 1])
                    abase = A1X if lat else A1C
                    w = 0 if lat else 1
                    for half in range(2):
                        pt, ptk = nps()
                        for c4 in range(4):
                            c = half * 4 + c4
                            tr(pt[:, c4 * 128:(c4 + 1) * 128], y[:, c * 128:(c + 1) * 128], [yk], [ptk])
                        for c4 in range(4):
                            c = half * 4 + c4
                            if half == 0:
                                ts("dve", xnt(c, t * 128, 128), pt[:, c4 * 128:(c4 + 1) * 128], col(abase, c), modcol(c, w),
                                   ALU.mult, ALU.add, [ptk, "cols", "modt"], [xk(t)])
                            else:
                                act(xnt(c, t * 128, 128), pt[:, c4 * 128:(c4 + 1) * 128], AF.Identity,
                                    [ptk, "cols", "modt"], [xk(t)], bias=modcol(c, w), scale=col(abase, c))
                for t in range(NT):
                    dma("actq", ACC[:, t * D:(t + 1) * D], x_d[t * 128:(t + 1) * 128, :], (), [acck(t)], "xin%d" % t)
                    dma("actq", POS[t % 2][:], pos_d[t * 128:(t + 1) * 128, :], (), ["pos%d" % (t % 2)], "pos%d" % (t % 2))
                    tt("pool", ACC[:, t * D:(t + 1) * D], ACC[:, t * D:(t + 1) * D], POS[t % 2][:], ALU.add,
                       [acck(t), "pos%d" % (t % 2)], [acck(t)])
                    act(SQJ[:], ACC[:, t * D:(t + 1) * D], AF.Square, [acck(t)], ["sqj", "ss%d" % t], accum=SS[:, t:t + 1])
                    if t >= 1:
                        norm_tile(t - 1)
                for i in range(2):
                    dma("actq", XC[i][:], ctx_d[i * 128:(i + 1) * 128, :], (), ["xc%d" % i], "xc%d" % i)
                    act(SQJ[:], XC[i][:], AF.Square, ["xc%d" % i], ["sqj", "ss%d" % (NT + i)], accum=SS[:, NT + i:NT + i + 1])
                norm_tile(NT - 1)
                norm_tile(NT)
                norm_tile(NT + 1)
                tt("dve", LBBC[:], LBR[:, 0:1024], LBR[:, 1024:2048], ALU.subtract, ["lbr"], ["lbbc"])
                act(LBBC[:], LBBC[:], AF.Sigmoid, ["lbbc"], ["lbbc"])
                ts("dve", OMLBC[:], LBBC[:], -1.0, 1.0, ALU.mult, ALU.add, ["lbbc"], ["omlbc"])
                for c in range(8):
                    if c % 4 == 0:
                        pb, pbk = nps()
                    dg = DG2[c % 2]
                    ts("dve", dg[:], IDENT[:], modcol(16 + c, 0), None, ALU.mult, None, ["ident", "modt"], ["dgb%d" % (c % 2)])
                    mm(pb[:, (c % 4) * 128:(c % 4 + 1) * 128], ONES[:], dg[:], True, True, ["ones", "dgb%d" % (c % 2)], [pbk])
                    if c % 4 == 3:
                        cp("dve", MX2BC[:, (c - 3) * 128:(c + 1) * 128], pb[:], [pbk], ["mx2bc"])
                P.emit()

            win_v = win_d.rearrange("(kc p) n -> p kc n", p=128)
            wout_v = wout_d.rearrange("(c p) n -> p c n", p=128)

            with ExitStack() as sn:
              if stop_after != "1A":
                G2 = "G:p2"
                WINF = sb("WINF", [128, 8 * 512], BF16, stack=sn)
                FW = sb("FW", [128, 512], stack=sn)
                DFTC = sb("DFTC", [128, 256], stack=sn)
                CSW = sb("CSW", [128, 4 * 256], BF16, stack=sn)
                WOS = [sb("WOS%d" % i, [128, D], stack=sn) for i in range(1)]
                WOUTF = sb("WOUTF", [128, 4 * D], BF16, stack=sn)
                UT = [sb("UT%d" % i, [128, L], BF16, stack=sn) for i in range(1)]
                AB = sb("AB", [128, 16 * 4 * 256], BF16, stack=sn)
                DF = [sb("DF%d" % i, [128, 4096], BF16, stack=sn) for i in range(2)]
                FXT = sb("FXT", [128, 4 * 512], BF16, stack=sn)
                dma("sp", FW[:], fw_d, (), ["fw"], G2)
                dma("sp", DFTC[:], dftc_d, (), ["dftc"], G2)
                dma("poolq", WINF[:].rearrange("p (kc n) -> p kc n", kc=8), win_v[:, :, 2560:3072], (), ["winf"], "winf")
                for g in range(4):
                    dma("sp", WOS[0][:], wout_v[:, 4 + g, :], (), ["wos0"], "wos0")
                    tt("dve", WOUTF[:, g * D:(g + 1) * D], WOS[0][:], MX2BC[:], ALU.mult,
                       ["wos0", "mx2bc"], ["woutf"])
                for g in range(4):
                    pc, pck = nps()
                    mm(pc[:, 0:128], DFTC[:, 0:128], FW[:, g * 128:(g + 1) * 128], True, True, ["dftc", "fw"], [pck])
                    mm(pc[:, 128:256], DFTC[:, 128:256], FW[:, g * 128:(g + 1) * 128], True, True, ["dftc", "fw"], [pck])
                    cp("dve", CSW[:, g * 256:(g + 1) * 256], pc[:, 0:256], [pck], ["csw"])
                for g in range(4):
                    u = UT[0]
                    uk = "ut0"
                    for t4 in range(4):
                        pu, puk = nps()
                        for kc in range(8):
                            mm(pu[:], WINF[:, kc * 512 + g * 128:kc * 512 + (g + 1) * 128], xnt(kc, t4 * 512, 512),
                               kc == 0, kc == 7, ["winf"] + [xk(t4 * 4 + i) for i in range(4)], [puk])
                        act(u[:, t4 * 512:(t4 + 1) * 512], pu[:], AF.Copy, [puk], [uk])
                    for j2 in range(8):
                        pa, pak = nps()
                        for jj in range(2):
                            j = j2 * 2 + jj
                            mm(pa[:, jj * 256:(jj + 1) * 256], u[:, j * 128:(j + 1) * 128], CSW[:, g * 256:(g + 1) * 256],
                               True, True, [uk, "csw"], [pak])
                        for jj in range(2):
                            j = j2 * 2 + jj
                            o_ap = AB[:, (j * 4 + g) * 256:(j * 4 + g + 1) * 256]
                            if j2 % 2 == 0:
                                cp("dve", o_ap, pa[:, jj * 256:(jj + 1) * 256], [pak], ["ab"])
                            else:
                                act(o_ap, pa[:, jj * 256:(jj + 1) * 256], AF.Copy, [pak], ["ab"])
                pcs = 0
                for lt in range(4):
                    banks = [nps() for _ in range(4)]
                    for cs in range(2):
                        for jh in range(2):
                            sl = pcs % 2
                            dma("sp", DF[sl][:], dftl_d[lt * 4 + cs * 2 + jh], (), ["df%d" % sl], "df%d" % sl)
                            pcs += 1
                            for g in range(4):
                                pg, pgk = banks[g]
                                for j8 in range(8):
                                    j = jh * 8 + j8
                                    first = (cs == 0 and jh == 0 and j8 == 0)
                                    last = (cs == 1 and jh == 1 and j8 == 7)
                                    mm(pg[:], AB[:, (j * 4 + g) * 256 + cs * 128:(j * 4 + g) * 256 + (cs + 1) * 128],
                                       DF[sl][:, j8 * 512:(j8 + 1) * 512], first, last, ["ab", "df%d" % sl], [pgk])
                    for g in range(4):
                        pg, pgk = banks[g]
                        if g % 2 == 0:
                            act(FXT[:, g * 512:(g + 1) * 512], pg[:], AF.Copy, [pgk], ["fxt"])
                        else:
                            cp("dve", FXT[:, g * 512:(g + 1) * 512], pg[:], [pgk], ["fxt"])
                    for tk in range(4):
                        t = lt * 4 + tk
                        for dh in range(2):
                            po, pok = nps()
                            for g in range(4):
                                mm(po[:], FXT[:, g * 512 + tk * 128:g * 512 + (tk + 1) * 128],
                                   WOUTF[:, g * D + dh * 512:g * D + (dh + 1) * 512], g == 0, g == 3, ["fxt", "woutf"], [pok])
                            a_ap = ACC[:, t * D + dh * 512:t * D + (dh + 1) * 512]
                            tt("dve", a_ap, po[:], a_ap, ALU.add, [pok, acck(t)], [acck(t)])
                P.emit()


            with ExitStack() as sh:
              if stop_after not in ("1A", "1N"):
                OTACC = sb("OTACC", [128, 2 * L], stack=sh)
                WQ = sb("WQ", [128, 8 * 256], BF16, stack=sh)
                WV = sb("WV", [128, 8 * 256], BF16, stack=sh)
                WG = sb("WG", [128, 8 * 256], BF16, stack=sh)
                WZ = [sb("WZ%d" % i, [128, 8 * 256], BF16, stack=sh) for i in range(2)]
                WOS2 = sb("WOSb", [128, D], stack=sh)
                WOUTH = sb("WOUTH", [128, 2 * D], BF16, stack=sh)

                def pair(name, dt=F32, w=256):
                    return [sb("%s_%d" % (name, i), [128, w], dt, stack=sh) for i in range(2)]
                T1 = pair("T1"); LOGF = pair("LOGF"); KTM = pair("KTM"); T2 = pair("T2"); KT = pair("KT")
                EPI = pair("EPI", F32, 512); EM = pair("EM"); ERD = pair("ERD", F32, 260)
                VBF = [sb("VBF_%d" % i, [128, 256], BF16, stack=sh) for i in range(3)]; QI = pair("QI", BF16); QE = pair("QE", BF16); KI = pair("KI", BF16)
                KP0 = pair("KP0", BF16); KP1 = pair("KP1", BF16); ATM = pair("ATM", BF16)
                S32 = sb("S32", [128, 256], stack=sh)
                SBF = [sb("SBF%d" % i, [128, 256], BF16, stack=sh) for i in range(2)]
                OO = sb("OO", [128, 256], stack=sh)
                SQ = sb("SQ", [128, 256], stack=sh)
                RS = sb("RS", [128, 256], stack=sh)
                SG = sb("SG", [128, 256], stack=sh)
                HXB = sb("HXB", [128, 256], BF16, stack=sh)
                MC = [CM[:, 0:128], CM[:, 384:512]]
                MI = [CM[:, 128:256], CM[:, 512:640]]
                M2 = [CM[:, 256:384], CM[:, 640:768]]
                SEL = CM[:, 768:770]
                MASK = [CM[:, 770:834], CM[:, 834:898]]
                for p in range(2):
                    memset("dve", ATM[p][:], 0.0, ["atm%d" % p])
                    memset("dve", KP0[p][:], 0.0, ["kp%d" % p])
                    memset("dve", KP1[p][:], 0.0, ["kp%d" % p])

                from collections import deque
                freeps = deque(range(8))

                def aps():
                    i = freeps.popleft()
                    return PS[i], "ps%d" % i

                def rps(key):
                    freeps.append(int(key[2:]))

                def com(it):
                    hp, dr, t, p = it["hp"], it["dr"], it["t"], it["p"]
                    return hp, dr, t, p, t < NT, t * 128, [xk(t)], "%d" % p, "vbf%d" % it["p3"], VBF[it["p3"]]

                def F1a(it):
                    hp, dr, t, p, lat, tok0, xr, P_, vk, vb = com(it)
                    wz, wzk = WZ[dr], "wz%d" % dr
                    lbo = dr * 512 + hp * 256
                    pzv, pzvk = aps()
                    for kc in range(8):
                        mm(pzv[:, 0:256], xnt(kc, tok0, 128), wz[:, kc * 256:(kc + 1) * 256], kc == 0, kc == 7,
                           xr + [wzk], [pzvk])
                    for kc in range(8):
                        mm(pzv[:, 256:512], xnt(kc, tok0, 128), WV[:, kc * 256:(kc + 1) * 256], kc == 0, kc == 7,
                           xr + ["wv"], [pzvk])
                    act(T1[p][:], pzv[:, 0:256], AF.Sigmoid, [pzvk], ["t1" + P_])
                    act(vb[:], pzv[:, 256:512], AF.Copy, [pzvk], [vk])
                    rps(pzvk)
                    tt("dve", T1[p][:], T1[p][:], OMLBC[:, lbo:lbo + 256], ALU.mult, ["t1" + P_, "omlbc"], ["t1" + P_])
                    tt("dve", T1[p][:], T1[p][:], LBBC[:, lbo:lbo + 256], ALU.add, ["t1" + P_, "lbbc"], ["t1" + P_])
                    act(LOGF[p][:], T1[p][:], AF.Ln, ["t1" + P_], ["logf" + P_])
                    ts("dve", KTM[p][:], T1[p][:], -1.0, 1.0, ALU.mult, ALU.add, ["t1" + P_], ["ktm" + P_])

                def F1b(it):
                    hp, dr, t, p, lat, tok0, xr, P_, vk, vb = com(it)
                    if not lat:
                        return
                    wz, wzk = WZ[dr], "wz%d" % dr
                    pzq, pzqk = aps()
                    for i in range(2):
                        for kc in range(8):
                            mm(pzq[:, i * 128:(i + 1) * 128], wz[:, kc * 256 + i * 128:kc * 256 + (i + 1) * 128],
                               xnt(kc, tok0, 128), kc == 0, kc == 7, xr + [wzk], [pzqk])
                    for i in range(2):
                        for kc in range(8):
                            mm(pzq[:, 256 + i * 128:256 + (i + 1) * 128], WQ[:, kc * 256 + i * 128:kc * 256 + (i + 1) * 128],
                               xnt(kc, tok0, 128), kc == 0, kc == 7, xr + ["wq"], [pzqk])
                    act(T2[p][:], pzq[:, 0:256], AF.Sigmoid, [pzqk], ["t2" + P_], scale=-1.0)
                    for i in range(2):
                        ts("dve", KT[p][:, i * 128:(i + 1) * 128], T2[p][:, i * 128:(i + 1) * 128],
                           col(OMLT, dr * 4 + hp * 2 + i), None, ALU.mult, None, ["t2" + P_, "cols"], ["kt" + P_])
                    it["pzq"] = (pzq, pzqk)

                def F2a(it):
                    hp, dr, t, p, lat, tok0, xr, P_, vk, vb = com(it)
                    pr, prk = aps()
                    mm(pr[:, 0:256], M2[dr], LOGF[p][:], True, True, ["cm", "logf" + P_], [prk])
                    for i in range(2):
                        mm(pr[:, 256 + i * 2:256 + i * 2 + 2], LOGF[p][:, i * 128:(i + 1) * 128], SEL, True, True,
                           ["cm", "logf" + P_], [prk])
                    if lat:
                        pcm, pcmk = aps()
                        for i in range(2):
                            mm(pcm[:, i * 128:(i + 1) * 128], LOGF[p][:, i * 128:(i + 1) * 128], MC[dr], True, True,
                               ["cm", "logf" + P_], [pcmk])
                        for i in range(2):
                            mm(pcm[:, 256 + i * 128:256 + (i + 1) * 128], LOGF[p][:, i * 128:(i + 1) * 128], MI[dr], True, True,
                               ["cm", "logf" + P_], [pcmk])
                    act(ERD[p][:], pr[:, 0:260], AF.Exp, [prk], ["er" + P_, "dec" + P_])
                    rps(prk)
                    if lat:
                        act(EPI[p][:], pcm[:, 0:512], AF.Exp, [pcmk], ["ep" + P_, "ei" + P_])
                        rps(pcmk)
                        P.op("dve", lambda e: e.reciprocal(EM[p][:], EPI[p][:, 0:256]), ["ep" + P_], ["em" + P_])

                def F2b(it):
                    hp, dr, t, p, lat, tok0, xr, P_, vk, vb = com(it)
                    tt("dve", KP0[p][0:64, :], KTM[p][0:64, :], ERD[p][0:64, 0:256], ALU.mult, ["ktm" + P_, "er" + P_], ["kp" + P_])
                    tt("dve", KP1[p][64:128, :], KTM[p][64:128, :], ERD[p][64:128, 0:256], ALU.mult, ["ktm" + P_, "er" + P_], ["kp" + P_])
                    pu, puk = aps()
                    for c in range(2):
                        kp = KP0[p] if c == 0 else KP1[p]
                        for i in range(2):
                            mm(pu[:, c * 256 + i * 128:c * 256 + (i + 1) * 128], kp[:, i * 128:(i + 1) * 128],
                               vb[:, i * 128:(i + 1) * 128], True, True, ["kp" + P_, vk], [puk])
                    it["pu"] = (pu, puk)
                    if not lat:
                        return
                    pzq, pzqk = it["pzq"]
                    tt("dve", QI[p][:], pzq[:, 256:512], EPI[p][:, 0:256], ALU.mult, [pzqk, "ep" + P_], ["qi" + P_])
                    tt("dve", KI[p][:], KT[p][:], EM[p][:], ALU.mult, ["kt" + P_, "em" + P_], ["ki" + P_])
                    tt("dve", QE[p][:], pzq[:, 256:512], EPI[p][:, 256:512], ALU.mult, [pzqk, "ei" + P_], ["qe" + P_])
                    rps(pzqk)
                    pa, pak = aps()
                    for i in range(2):
                        mm(pa[:, i * 128:(i + 1) * 128], KI[p][:, i * 128:(i + 1) * 128], QI[p][:, i * 128:(i + 1) * 128],
                           True, True, ["ki" + P_, "qi" + P_], [pak])
                    for i in range(2):
                        for c in range(2):
                            r0, r1 = c * 64, (c + 1) * 64
                            tt("dve", ATM[p][r0:r1, i * 128 + c * 64:i * 128 + (c + 1) * 64],
                               pa[r0:r1, i * 128 + c * 64:i * 128 + (c + 1) * 64], MASK[dr][r0:r1, :], ALU.mult,
                               [pak, "cm"], ["atm" + P_])
                    rps(pak)

                def chunk_step(it, ci):
                    hp, dr, t, p, lat, tok0, xr, P_, vk, vb = com(it)
                    pu, puk = it["pu"]
                    c = ((0, 1) if dr == 0 else (1, 0))[ci]
                    sidx = it["sidx"]
                    if lat:
                        for i in range(2):
                            po, pok = it["pos"][i]
                            mm(po[:, c * 64:(c + 1) * 64], SBF[sidx][:, i * 128:(i + 1) * 128],
                               QE[p][:, i * 128 + c * 64:i * 128 + (c + 1) * 64], False, ci == 1,
                               ["sbf%d" % sidx, "qe" + P_], [pok])
                    for i in range(2):
                        stt("dve", S32[:, i * 128:(i + 1) * 128], S32[:, i * 128:(i + 1) * 128],
                            ERD[p][:, 256 + i * 2 + c:256 + i * 2 + c + 1], pu[:, c * 256 + i * 128:c * 256 + (i + 1) * 128],
                            ALU.mult, ALU.add, ["s32", "dec" + P_, puk], ["s32"])
                    nidx = 1 - sidx
                    act(SBF[nidx][:], S32[:], AF.Copy, ["s32"], ["sbf%d" % nidx])
                    it["sidx"] = nidx

                def Ba(it):
                    hp, dr, t, p, lat, tok0, xr, P_, vk, vb = com(it)
                    it["sidx"] = 0
                    if lat:
                        it["pos"] = [aps() for _ in range(2)]
                        for i in range(2):
                            po, pok = it["pos"][i]
                            mm(po[:, 0:128], vb[:, i * 128:(i + 1) * 128], ATM[p][:, i * 128:(i + 1) * 128], True, False,
                               [vk, "atm" + P_], [pok])
                    chunk_step(it, 0)

                def Bb(it):
                    hp, dr, t, p, lat, tok0, xr, P_, vk, vb = com(it)
                    chunk_step(it, 1)
                    rps(it["pu"][1])
                    if not lat:
                        return
                    if dr == 0:
                        for i in range(2):
                            po, pok = it["pos"][i]
                            cp("dve", OTACC[:, i * L + tok0:i * L + tok0 + 128], po[:, 0:128], [pok], ["otacc%d" % t])
                            rps(pok)
                        return
                    for i in range(2):
                        po, pok = it["pos"][i]
                        tt("dve", OO[:, i * 128:(i + 1) * 128], po[:, 0:128], OTACC[:, i * L + tok0:i * L + tok0 + 128],
                           ALU.add, [pok, "otacc%d" % t], ["oo"])
                        rps(pok)
                    act(SQ[:], OO[:], AF.Square, ["oo"], ["sq"])

                def Bc(it):
                    hp, dr, t, p, lat, tok0, xr, P_, vk, vb = com(it)
                    if not lat or dr == 0:
                        return
                    pss, pssk = aps()
                    for i in range(2):
                        for kc in range(8):
                            mm(pss[:, 256 + i * 128:256 + (i + 1) * 128], WG[:, kc * 256 + i * 128:kc * 256 + (i + 1) * 128],
                               xnt(kc, tok0, 128), kc == 0, kc == 7, xr + ["wg"], [pssk])
                    mm(pss[:, 0:256], ONES[:], SQ[:], True, True, ["ones", "sq"], [pssk])
                    act(RS[:], pss[:, 0:256], AF.Sqrt, [pssk, "epsc"], ["rs"], bias=EPSC[:, 0:1], scale=1.0 / 128)
                    act(SG[:], pss[:, 256:512], AF.Sigmoid, [pssk], ["sg"])
                    rps(pssk)
                    P.op("dve", lambda e: e.reciprocal(RS[:], RS[:]), ["rs"], ["rs"])
                    tt("dve", OO[:], OO[:], RS[:], ALU.mult, ["oo", "rs"], ["oo"])
                    stt("dve", HXB[:], OO[:], COLS[:, GNC:GNC + 1], SG[:], ALU.mult, ALU.mult, ["oo", "gnc", "sg"], ["hxb"])
                    for dh in range(2):
                        px, pxk = aps()
                        for i in range(2):
                            mm(px[:], HXB[:, i * 128:(i + 1) * 128], WOUTH[:, i * D + dh * 512:i * D + (dh + 1) * 512],
                               i == 0, i == 1, ["hxb", "wouth"], [pxk])
                        a_ap = ACC[:, t * D + dh * 512:t * D + (dh + 1) * 512]
                        tt("dve", a_ap, px[:], a_ap, ALU.add, [pxk, acck(t)], [acck(t)])
                        rps(pxk)

                itn = 0
                for hp in range(2):
                    c0 = hp * 256

                    def wload(dst, colbase, key):
                        dma("poolq", dst[:].rearrange("p (kc n) -> p kc n", kc=8), win_v[:, :, colbase + c0:colbase + c0 + 256],
                            (), [key], key)
                    wload(WZ[0], 1536, "wz0")
                    wload(WV, 512, "wv")
                    wload(WQ, 0, "wq")
                    wload(WZ[1], 2048, "wz1")
                    wload(WG, 1024, "wg")
                    for i in range(2):
                        dma("sp", WOS2[:], wout_v[:, hp * 2 + i, :], (), ["wosb"], "wosb")
                        tt("pool", WOUTH[:, i * D:(i + 1) * D], WOS2[:], MX2BC[:], ALU.mult, ["wosb", "mx2bc"], ["wouth"])
                    for dr in range(2):
                        memset("dve", S32[:], 0.0, ["s32"])
                        memset("dve", SBF[0][:], 0.0, ["sbf0"])
                        order = [16, 17] + list(range(16)) if dr == 0 else [17, 16] + list(range(15, -1, -1))
                        its = []
                        for t in order:
                            its.append({"hp": hp, "dr": dr, "t": t, "p": itn % 2, "p3": itn % 3, "cur": 0})
                            itn += 1
                        ni = len(its)
                        F1a(its[0]); F1b(its[0])
                        F1a(its[1]); F1b(its[1])
                        F2a(its[0]); F2b(its[0])
                        for n in range(ni):
                            n1 = its[n + 1] if n + 1 < ni else None
                            n2 = its[n + 2] if n + 2 < ni else None
                            if n1: F2a(n1)
                            Ba(its[n])
                            if n2: F1a(n2)
                            if n2: F1b(n2)
                            if n1: F2b(n1)
                            Bb(its[n])
                            Bc(its[n])
                P.emit()

        with ExitStack() as s2:
            HMT = sb("HMT", [128, 8 * L], BF16, stack=s2)
            SS2 = sb("SS2", [128, NT], stack=s2)
            RSTD2 = sb("RSTD2", [128, NT], stack=s2)
            B1T = sb("B1T", [128, NE * 16], stack=s2)

            def hk(t):
                return "hmt%d" % t

            if stop_after is None or stop_after in ("2", "3"):
                with ExitStack() as sr:
                    G3 = "G:p3"
                    RW = sb("RW", [128, 256], stack=sr)
                    RBBC = sb("RBBC", [128, NE], stack=sr)
                    B2P = sb("B2P", [NE, D], stack=sr)
                    SQJ2 = sb("SQJ2", [128, D], BF16, stack=sr)
                    Y2 = [sb("Y2_%d" % i, [128, D], stack=sr) for i in range(2)]
                    HM32 = [sb("HM32_%d" % i, [128, D], stack=sr) for i in range(2)]
                    LG = sb("LG", [128, NE], stack=sr)
                    M8 = sb("M8", [128, 8], stack=sr)
                    NEGM = sb("NEGM", [128, 1], stack=sr)
                    EX = sb("EX", [128, NE], stack=sr)
                    MSK = sb("MSK", [128, NE], stack=sr)
                    SUMC = sb("SUMC", [128, 1], stack=sr)
                    GT = [sb("GT%d" % i, [NE, 128], stack=sr) for i in range(2)]
                    dma("sp", RW[:], rw_d, (), ["rw"], G3)
                    dma("sp", RBBC[:], rb_d.partition_broadcast(128), (), ["rbbc"], G3)
                    dma("sp", B2P[:], b2_d, (), ["b2p"], G3)
                    dma("sp", B1T[:], b1_d, (), ["b1t"], G3)
                    tt("dve", B2P[:], B2P[:], MX5BC[0:NE, :], ALU.mult, ["b2p", "mx5bc"], ["b2p"])
                    b13 = B1T[:].rearrange("p (e c) -> p e c", c=16)
                    ts("dve", b13[:, :, 8:16], b13[:, :, 8:16], 1.0, None, ALU.add, None, ["b1t"], ["b1t"])
                    for t in range(NT):
                        act(SQJ2[:], ACC[:, t * D:(t + 1) * D], AF.Square, [acck(t)], ["sqj2", "ss2"], accum=SS2[:, t:t + 1])
                    act(RSTD2[:], SS2[:], AF.Sqrt, ["ss2", "epsc"], ["rstd2"], bias=EPSC[:, 0:1], scale=1.0 / D)
                    P.op("dve", lambda e: e.reciprocal(RSTD2[:], RSTD2[:]), ["rstd2"], ["rstd2"])
                    def stA(t):
                        y = Y2[t % 2]
                        yk = "y2_%d" % (t % 2)
                        h32 = HM32[t % 2]
                        h32k = "hm32_%d" % (t % 2)
                        act(y[:], ACC[:, t * D:(t + 1) * D], AF.Copy, [acck(t), "rstd2"], [yk], scale=RSTD2[:, t:t + 1])
                        for half in range(2):
                            pt, ptk = nps()
                            for c4 in range(4):
                                c = half * 4 + c4
                                tr(pt[:, c4 * 128:(c4 + 1) * 128], y[:, c * 128:(c + 1) * 128], [yk], [ptk])
                            for c4 in range(4):
                                c = half * 4 + c4
                                if half == 0:
                                    ts("dve", h32[:, c * 128:(c + 1) * 128], pt[:, c4 * 128:(c4 + 1) * 128], col(A2C, c),
                                       modcol(24 + c, 0), ALU.mult, ALU.add, [ptk, "cols", "modt"], [h32k])
                                else:
                                    act(h32[:, c * 128:(c + 1) * 128], pt[:, c4 * 128:(c4 + 1) * 128], AF.Identity,
                                        [ptk, "cols", "modt"], [h32k], bias=modcol(24 + c, 0), scale=col(A2C, c))
                        for c in range(8):
                            eng = "pool" if c % 2 == 0 else "act"
                            if eng == "pool":
                                cp("pool", HMT[:, c * L + t * 128:c * L + (t + 1) * 128], h32[:, c * 128:(c + 1) * 128], [h32k], [hk(t)])
                            else:
                                act(HMT[:, c * L + t * 128:c * L + (t + 1) * 128], h32[:, c * 128:(c + 1) * 128], AF.Copy, [h32k], [hk(t)])
                        pl, plk = nps()
                        for c in range(8):
                            mm(pl[:, 0:NE], h32[:, c * 128:(c + 1) * 128], RW[:, c * NE:(c + 1) * NE], c == 0, c == 7,
                               [h32k, "rw"], [plk])
                        return (h32, h32k, pl, plk)
                    def stB(t, st):
                        h32, h32k, pl, plk = st
                        tt("dve", LG[:], pl[:, 0:NE], RBBC[:], ALU.add, [plk, "rbbc"], ["lg"])
                        P.op("dve", lambda e: e.max(out=M8[:], in_=LG[:]), ["lg"], ["m8"])
                        ts("dve", NEGM[:], M8[:, 0:1], -1.0, None, ALU.mult, None, ["m8"], ["negm"])
                        act(EX[:], LG[:], AF.Exp, ["lg", "negm"], ["ex"], bias=NEGM[:, 0:1], scale=1.0)
                        ts("dve", MSK[:], LG[:], M8[:, 3:4], None, ALU.is_ge, None, ["lg", "m8"], ["msk"])
                        tt("dve", EX[:], EX[:], MSK[:], ALU.mult, ["ex", "msk"], ["ex"])
                        P.op("dve", lambda e: e.tensor_reduce(SUMC[:], EX[:], AX.X, ALU.add), ["ex"], ["sumc"])
                        P.op("dve", lambda e: e.reciprocal(SUMC[:], SUMC[:]), ["sumc"], ["sumc"])
                        g_ap = GATE[:, t * NE:(t + 1) * NE]
                        ts("dve", g_ap, EX[:], SUMC[:, 0:1], None, ALU.mult, None, ["ex", "sumc"], ["gate%d" % t])
                        pgt, pgtk = nps()
                        tr(pgt[0:NE, 0:128], g_ap, ["gate%d" % t], [pgtk])
                        gt = GT[t % 2]
                        gtk = "gt%d" % (t % 2)
                        act(gt[:], pgt[0:NE, 0:128], AF.Copy, [pgtk], [gtk])
                        for dh in range(2):
                            pb2, pb2k = nps()
                            mm(pb2[:], gt[:], B2P[:, dh * 512:(dh + 1) * 512], True, True, [gtk, "b2p"], [pb2k])
                            a_ap = ACC[:, t * D + dh * 512:t * D + (dh + 1) * 512]
                            tt("dve", a_ap, pb2[:], a_ap, ALU.add, [pb2k, acck(t)], [acck(t)])
                    stq = {0: stA(0)}
                    for t in range(NT):
                        if t + 1 < NT:
                            stq[t + 1] = stA(t + 1)
                        stB(t, stq.pop(t))
                    P.emit()

            if stop_after is None or stop_after == "3":
                with ExitStack() as sm:
                    HHT = sb("HHT", [128, 8 * L], BF16, stack=sm)
                    W1S = [sb("W1S%d" % i, [128, 2048], stack=sm) for i in range(2)]
                    W1B = [sb("W1B%d" % i, [128, 2048], BF16, stack=sm) for i in range(2)]
                    W2S = [sb("W2S%d" % i, [128, D], stack=sm) for i in range(2)]
                    W2B = sb("W2B", [128, 8 * D], BF16, stack=sm)
                    AG = [sb("AG%d" % i, [128, 512], stack=sm) for i in range(2)]
                    SGM = [sb("SGM%d" % i, [128, 512], stack=sm) for i in range(2)]
                    TL = [sb("TL%d" % i, [128, 512], stack=sm) for i in range(2)]
                    QQ = [sb("QQ%d" % i, [128, 512], stack=sm) for i in range(2)]
                    allh = [hk(t) for t in range(NT)]
                    npc = NE * 8

                    def issue_loads(n):
                        e, fc = divmod(n, 8)
                        s = n % 2
                        dma("sp", W1S[s][:], w1_d[n], (), ["w1s%d" % s], "w1s%d" % s)
                        dma("sp", W2S[s][:], w2_d[n], (), ["w2s%d" % s], "w2s%d" % s)

                    def cast_w1(m):
                        sm = m % 2
                        act(W1B[sm][:], W1S[sm][:], AF.Copy, ["w1s%d" % sm], ["w1b%d" % sm])

                    issue_loads(0)
                    cast_w1(0)
                    ucnt = 0
                    for n in range(npc):
                        e, fc = divmod(n, 8)
                        s = n % 2
                        if n + 1 < npc:
                            issue_loads(n + 1)
                        tt("pool", W2B[:, fc * D:(fc + 1) * D], W2S[s][:], MX5BC[:], ALU.mult,
                           ["w2s%d" % s, "mx5bc"], ["w2b%d" % fc])
                        for t4 in range(4):
                            u = ucnt % 2
                            ucnt += 1
                            hr = [hk(t4 * 4 + i) for i in range(4)]
                            pgl = []
                            for half in range(2):
                                pb, pbk = nps()
                                pgl.append((pb, pbk))
                                for kc in range(8):
                                    mm(pb[:], W1B[s][:, kc * 256 + half * 128:kc * 256 + (half + 1) * 128],
                                       HMT[:, kc * L + t4 * 512:kc * L + (t4 + 1) * 512], kc == 0, kc == 7,
                                       ["w1b%d" % s] + hr, [pbk])
                            (pgp, pgk), (plp, plk) = pgl
                            b1g = B1T[:, e * 16 + fc:e * 16 + fc + 1]
                            b1l = B1T[:, e * 16 + 8 + fc:e * 16 + 8 + fc + 1]
                            ts("dve", AG[u][:], pgp[:], b1g, 7.0, ALU.add, ALU.min, [pgk, "b1t"], ["ag%d" % u])
                            act(SGM[u][:], AG[u][:], AF.Sigmoid, ["ag%d" % u], ["sgm%d" % u], scale=1.702)
                            ts("dve", TL[u][:], plp[:], b1l, 8.0, ALU.add, ALU.min, [plk, "b1t"], ["tl%d" % u])
                            stt("dve", QQ[u][:], TL[u][:], -6.0, AG[u][:], ALU.max, ALU.mult, ["tl%d" % u, "ag%d" % u], ["qq%d" % u])
                            tt("pool", HHT[:, fc * L + t4 * 512:fc * L + (t4 + 1) * 512], QQ[u][:], SGM[u][:], ALU.mult,
                               ["qq%d" % u, "sgm%d" % u], ["hht%d_%d" % (fc, t4)])
                            if t4 == 0 and n + 1 < npc:
                                cast_w1(n + 1)
                        if fc == 7:
                            for tk in range(NT):
                                for dh in range(2):
                                    po, pok = nps()
                                    for f in range(8):
                                        mm(po[:], HHT[:, f * L + tk * 128:f * L + (tk + 1) * 128],
                                           W2B[:, f * D + dh * 512:f * D + (dh + 1) * 512], f == 0, f == 7,
                                           ["hht%d_%d" % (f, tk // 4), "w2b%d" % f], [pok])
                                    a_ap = ACC[:, tk * D + dh * 512:tk * D + (dh + 1) * 512]
                                    stt("dve", a_ap, po[:], GATE[:, tk * NE + e:tk * NE + e + 1], a_ap, ALU.mult, ALU.add,
                                        [pok, "gate%d" % tk, acck(tk)], [acck(tk)])
                    P.emit()

            with ExitStack() as sf:
                G4 = "G:p4"
                FGBC = sb("FGBC", [128, D], stack=sf)
                SQJ3 = sb("SQJ3", [128, D], BF16, stack=sf)
                SS3 = sb("SS3", [128, NT], stack=sf)
                OUTT = [sb("OUTT%d" % i, [128, D], stack=sf) for i in range(2)]
                dma("sp", FGBC[:], fg_d.partition_broadcast(128), (), ["fgbc"], G4)
                final_norm = stop_after is None
                if final_norm:
                    for t in range(NT):
                        act(SQJ3[:], ACC[:, t * D:(t + 1) * D], AF.Square, [acck(t)], ["sqj3", "ss3"], accum=SS3[:, t:t + 1])
                    act(SS3[:], SS3[:], AF.Sqrt, ["ss3", "epsc"], ["ss3"], bias=EPSC[:, 0:1], scale=1.0 / D)
                    P.op("dve", lambda e: e.reciprocal(SS3[:], SS3[:]), ["ss3"], ["ss3"])
                for t in range(NT):
                    o = OUTT[t % 2]
                    ok = "outt%d" % (t % 2)
                    if final_norm:
                        stt("dve", o[:], ACC[:, t * D:(t + 1) * D], SS3[:, t:t + 1], FGBC[:], ALU.mult, ALU.mult,
                            [acck(t), "ss3", "fgbc"], [ok])
                    else:
                        cp("dve", o[:], ACC[:, t * D:(t + 1) * D], [acck(t)], [ok])
                    dma("sp", out_d[t * 128:(t + 1) * 128, :], o[:], [ok], (), "out%d" % (t % 2))
                P.out_keys = ["out0", "out1"]
                P.emit(final_wait_out=True)
    return nc


def _prep_inputs(x, c, ctx, c_ctx, ada_w, ada_b, norm1_g, w_in, hgrn_lb, hgrn_gnorm, fnet_w, w_out,
                 norm2_g, router_w, router_b, moe_w1, moe_b1, moe_w2, moe_b2, final_g):
    f = lambda a: np.ascontiguousarray(np.asarray(a, dtype=np.float32))
    cs = _get_consts()
    shared = {}
    shared["pos"] = cs["pos"]
    shared["ada_w"] = f(ada_w[0])
    shared["ada_bT"] = f(np.asarray(ada_b[0]).reshape(48, 128).T)
    shared["n1gT"] = f(np.asarray(norm1_g[0]).reshape(8, 128).T)
    shared["n2gT"] = f(np.asarray(norm2_g[0]).reshape(8, 128).T)
    shared["w_in"] = f(w_in[0])
    lb = np.asarray(hgrn_lb, dtype=np.float32)
    shared["lbT"] = f(lb.reshape(2, 2, 4, 128).transpose(3, 0, 1, 2).reshape(128, 16))
    shared["lbrow"] = f(lb.reshape(1, 2048))
    shared["gn"] = f(np.asarray(hgrn_gnorm[0]).reshape(128, 1))
    shared["fnet_w"] = f(np.asarray(fnet_w[0]).transpose(1, 0, 2).reshape(128, 512))
    shared["w_out"] = f(w_out[0])
    shared["rw"] = f(np.asarray(router_w[0]).reshape(8, 128, NE).transpose(1, 0, 2).reshape(128, 256))
    shared["rb"] = f(np.asarray(router_b[0]).reshape(1, NE))
    w1 = np.asarray(moe_w1[0], dtype=np.float32)
    w1r = w1.reshape(NE, 8, 128, 2, 8, 128).transpose(0, 4, 2, 1, 3, 5)
    shared["w1r"] = np.ascontiguousarray(w1r).reshape(NE * 8, 128, 2048)
    shared["b1T"] = f(np.asarray(moe_b1[0]).reshape(NE, 16, 128).transpose(2, 0, 1).reshape(128, NE * 16))
    shared["w2"] = f(moe_w2[0]).reshape(NE * 8, 128, D)
    shared["b2"] = f(moe_b2[0])
    shared["fg"] = f(np.asarray(final_g).reshape(1, D))
    shared["ident"] = cs["ident"]
    shared["ones"] = cs["ones"]
    shared["cm"] = cs["cm"]
    shared["dftc"] = cs["dftc"]
    shared["dftl"] = cs["dftl"]
    x = np.asarray(x, dtype=np.float32)
    c = np.asarray(c, dtype=np.float32)
    ctx = np.asarray(ctx, dtype=np.float32)
    c_ctx = np.asarray(c_ctx, dtype=np.float32)
    in_maps = []
    for b in range(8):
        m = dict(shared)
        m["x"] = np.ascontiguousarray(x[b])
        m["ctx"] = np.ascontiguousarray(ctx[b])
        cc = np.stack([c[b].reshape(8, 128).T, c_ctx.reshape(8, 128).T], axis=-1)
        m["cT"] = np.ascontiguousarray(cc.reshape(128, 16)).astype(np.float32)
        in_maps.append(m)
    return in_maps


_NC_CACHE = {}


def kernel(**inputs):
    in_maps = _prep_inputs(**inputs)
    if "nc" not in _NC_CACHE:
        _NC_CACHE["nc"] = build()
    nc = _NC_CACHE["nc"]
    res = run_bass_kernel_spmd(nc, in_maps, core_ids=list(range(8)))
    out = np.stack([np.asarray(r["out"], dtype=np.float32) for r in res.results], axis=0)
    return out.reshape(8, L, D)
```

```python
import numpy as np
import ml_dtypes
from contextlib import ExitStack
import concourse.bass as bass
import concourse.mybir as mybir
from concourse.bass_utils import run_bass_kernel_spmd

F32 = mybir.dt.float32
BF16 = mybir.dt.bfloat16
AF = mybir.ActivationFunctionType
ALU = mybir.AluOpType
AX = mybir.AxisListType

D = 1024
L = 2048
LC = 256
NT = 16
NTA = 18
NE = 32
EPS = 1e-6

STREAM_OF = {"pe": "pe", "act": "act", "dve": "dve", "pool": "pool",
             "sp": "sp", "poolq": "pool", "actq": "act"}


class Op:
    __slots__ = ("eng", "fn", "is_dma", "semkey", "phase", "seq", "needs_inc",
                 "inc_val", "waits", "dma_val", "stream")


class Prog:
    def __init__(self, nc, sem_alloc):
        self.nc = nc
        self.sem_alloc = sem_alloc
        self.phase = 0
        self.ops = []
        self.last_w = {}
        self.readers = {}
        self.eng_sem = {}
        self.eng_cnt = {s: 0 for s in ("pe", "act", "dve", "pool", "sp")}
        self.dma_sem = {}
        self.dma_cnt = {}
        self.waited = {}
        self.seqc = {s: 0 for s in ("pe", "act", "dve", "pool", "sp")}
        self.out_keys = []
        self.nops = 0

    def _sem_for_stream(self, s):
        if s not in self.eng_sem:
            self.eng_sem[s] = self.sem_alloc("e_" + s)
        return self.eng_sem[s]

    def _sem_for_key(self, k):
        if k not in self.dma_sem:
            self.dma_sem[k] = self.sem_alloc("d_" + str(k).replace(":", "_"))
            self.dma_cnt[k] = 0
        return self.dma_sem[k]

    def op(self, eng, fn, reads=(), writes=(), semkey=None):
        o = Op()
        o.eng = eng
        o.fn = fn
        o.is_dma = semkey is not None
        o.semkey = semkey
        o.phase = self.phase
        o.stream = STREAM_OF[eng]
        self.seqc[o.stream] += 1
        o.seq = self.seqc[o.stream]
        o.needs_inc = False
        o.inc_val = None
        o.waits = []
        o.dma_val = None
        if o.is_dma:
            self._sem_for_key(semkey)
            self.dma_cnt[semkey] += 16
            o.dma_val = self.dma_cnt[semkey]
        deps = []
        for k in reads:
            w = self.last_w.get(k)
            if w is not None:
                deps.append((w, True))
        for k in writes:
            w = self.last_w.get(k)
            if w is not None:
                deps.append((w, False))
            deps.extend((r, False) for r in self.readers.get(k, ()))
        best = {}
        seen_dma = set()
        for d, raw in deps:
            if d is o:
                continue
            if d.is_dma:
                if id(d) not in seen_dma:
                    seen_dma.add(id(d))
                    o.waits.append(("dma", d))
            else:
                if d.phase != self.phase:
                    continue
                if d.stream == o.stream and not o.is_dma:
                    if not raw or o.stream == "pe":
                        continue
                b = best.get(d.stream)
                if b is None or d.seq > b.seq:
                    best[d.stream] = d
        for s, d in best.items():
            o.waits.append(("eng", d))
        for k in reads:
            self.readers.setdefault(k, []).append(o)
        for k in writes:
            self.last_w[k] = o
            self.readers[k] = []
        self.ops.append(o)
        self.nops += 1
        return o

    def emit(self, final_wait_out=False):
        nc = self.nc
        ops = self.ops
        for o in ops:
            for kind, d in o.waits:
                if kind == "eng":
                    d.needs_inc = True
        for o in ops:
            if not o.is_dma and o.needs_inc:
                self._sem_for_stream(o.stream)
                self.eng_cnt[o.stream] += 1
                o.inc_val = self.eng_cnt[o.stream]
        by_stream = {s: [] for s in ("pe", "act", "dve", "pool", "sp")}
        for o in ops:
            by_stream[o.stream].append(o)
        waited = self.waited
        prog = self

        def run(stream, e):
            for o in by_stream[stream]:
                for kind, d in o.waits:
                    if kind == "dma":
                        sem = prog.dma_sem[d.semkey]
                        nm = ("d", d.semkey)
                        if str(d.semkey).startswith("G:"):
                            val = prog.dma_cnt[d.semkey]
                        else:
                            val = d.dma_val
                    else:
                        sem = prog.eng_sem[d.stream]
                        nm = ("e", d.stream)
                        val = d.inc_val
                    if waited.get((stream, nm), 0) >= val:
                        continue
                    waited[(stream, nm)] = val
                    e.wait_ge(sem, val)
                ins = o.fn(e)
                if o.is_dma:
                    ins.then_inc(prog.dma_sem[o.semkey], 16)
                elif o.needs_inc:
                    ins.then_inc(prog.eng_sem[o.stream], 1)
            if final_wait_out and stream == "sp":
                for k in prog.out_keys:
                    e.wait_ge(prog.dma_sem[k], prog.dma_cnt[k])

        with nc.Block() as block:
            if by_stream["sp"] or final_wait_out:
                @block.sync
                def _(e):
                    run("sp", e)
            if by_stream["pe"]:
                @block.tensor
                def _(e):
                    run("pe", e)
            if by_stream["act"]:
                @block.scalar
                def _(e):
                    run("act", e)
            if by_stream["dve"]:
                @block.vector
                def _(e):
                    run("dve", e)
            if by_stream["pool"]:
                @block.gpsimd
                def _(e):
                    run("pool", e)
        self.ops = []
        self.phase += 1


def _pos_table():
    quarter = D // 4
    omega = (1.0 / (np.float32(10000.0) ** (np.arange(quarter, dtype=np.float32) / np.float32(quarter)))).astype(np.float32)

    def axis_emb(n):
        ang = np.arange(n, dtype=np.float32)[:, None] * omega[None, :]
        return np.concatenate([np.sin(ang), np.cos(ang)], axis=-1).astype(np.float32)

    rows, cols = L // 64, 64
    er = np.broadcast_to(axis_emb(rows)[:, None, :], (rows, cols, D // 2))
    ec = np.broadcast_to(axis_emb(cols)[None, :, :], (rows, cols, D // 2))
    return np.ascontiguousarray(np.concatenate([er, ec], axis=-1).reshape(rows * cols, D)).astype(np.float32)


def _consts():
    c = {}
    c["pos"] = _pos_table()
    c["ident"] = np.eye(128, dtype=np.float32)
    c["ones"] = np.ones((128, 128), dtype=np.float32)
    s = np.arange(128)
    same = (s[:, None] // 64) == (s[None, :] // 64)
    si = (s % 64)[:, None]
    ti = (s % 64)[None, :]
    mi0 = same & (si <= ti)
    mc0 = same * ((si <= ti).astype(np.float32) - (si <= 31).astype(np.float32))
    m20 = same & (si > ti)
    mi1 = same & (si >= ti)
    mc1 = same * ((si >= ti).astype(np.float32) - (si >= 32).astype(np.float32))
    m21 = same & (si < ti)
    sel = np.zeros((128, 2), np.float32)
    sel[:64, 0] = 1
    sel[64:, 1] = 1
    p_in = (s % 64)[:, None]
    t_in = np.arange(64)[None, :]
    mask0 = (p_in <= t_in).astype(np.float32)
    mask1 = (p_in >= t_in).astype(np.float32)
    cm = np.concatenate([mc0, mi0, m20, mc1, mi1, m21, sel, mask0, mask1], axis=1).astype(np.float32)
    c["cm"] = np.ascontiguousarray(cm)
    k = np.arange(128, dtype=np.float64)
    ang = 2.0 * np.pi * np.outer(k, k) / 128.0
    c["dftc"] = np.ascontiguousarray(np.concatenate([np.cos(ang) / 512.0, -np.sin(ang) / 512.0], axis=1)).astype(np.float32)
    kk = np.arange(L, dtype=np.int64)
    ph = (np.outer(kk, kk) % L).astype(np.float64) * (2.0 * np.pi / L)
    mats = [np.cos(ph), np.sin(ph)]
    pieces = np.empty((4, 2, 2, 128, 8, 512), dtype=ml_dtypes.bfloat16)
    for cs in range(2):
        m = mats[cs].reshape(2, 8, 128, 4, 512)
        pieces[:, cs] = np.transpose(m, (3, 0, 2, 1, 4)).astype(ml_dtypes.bfloat16)
    c["dftl"] = np.ascontiguousarray(pieces.reshape(16, 128, 4096))
    return c


_CONSTS = None


def _get_consts():
    global _CONSTS
    if _CONSTS is None:
        _CONSTS = _consts()
    return _CONSTS


def build(stop_after=None):
    nc = bass.Bass("TRN2", target_bir_lowering=False)

    def din(name, shape, dt=F32):
        return nc.dram_tensor(name, list(shape), dt, kind="ExternalInput").ap()

    x_d = din("x", [L, D])
    pos_d = din("pos", [L, D])
    ctx_d = din("ctx", [LC, D])
    cT_d = din("cT", [128, 16])
    adaw_d = din("ada_w", [D, 6 * D])
    adab_d = din("ada_bT", [128, 48])
    n1g_d = din("n1gT", [128, 8])
    n2g_d = din("n2gT", [128, 8])
    win_d = din("w_in", [D, 3072])
    lbT_d = din("lbT", [128, 16])
    lbrow_d = din("lbrow", [1, 2048])
    gn_d = din("gn", [128, 1])
    fw_d = din("fnet_w", [128, 512])
    wout_d = din("w_out", [D, D])
    rw_d = din("rw", [128, 256])
    rb_d = din("rb", [1, 32])
    w1_d = din("w1r", [NE * 8, 128, 2048])
    b1_d = din("b1T", [128, NE * 16])
    w2_d = din("w2", [NE * 8, 128, D])
    b2_d = din("b2", [NE, D])
    fg_d = din("fg", [1, D])
    ident_d = din("ident", [128, 128])
    ones_d = din("ones", [128, 128])
    cm_d = din("cm", [128, 898])
    dftc_d = din("dftc", [128, 256])
    dftl_d = din("dftl", [16, 128, 4096], BF16)
    out_d = nc.dram_tensor("out", [L, D], F32, kind="ExternalOutput").ap()
    dbg_d = nc.dram_tensor("dbg", [128, 4608], F32, kind="ExternalOutput").ap() if stop_after is not None else None

    with ExitStack() as es:
        def sb(name, shape, dt=F32, stack=es):
            return stack.enter_context(nc.sbuf_tensor(name, list(shape), dt))

        def sem_alloc(name):
            return es.enter_context(nc.semaphore(name))

        P = Prog(nc, sem_alloc)
        PS = [es.enter_context(nc.psum_tensor("ps%d" % i, [128, 512], F32)) for i in range(8)]
        psi = [0]

        def nps():
            i = psi[0] % 8
            psi[0] += 1
            return PS[i], "ps%d" % i

        def dma(q, out, in_, reads, writes, key):
            P.op(q, lambda e: e.dma_start(out=out, in_=in_), reads, writes, semkey=key)

        def mm(out, lhsT, rhs, st, sp, reads, writes):
            P.op("pe", lambda e: e.matmul(out, lhsT, rhs, start=st, stop=sp), reads, writes)

        def tr(out, in_, reads, writes):
            P.op("pe", lambda e: e.transpose(out, in_, IDENT[:]), list(reads) + ["ident"], writes)

        def act(out, in_, func, reads, writes, bias=None, scale=None, accum=None):
            kw = {}
            if bias is not None:
                kw["bias"] = bias
            if scale is not None:
                kw["scale"] = scale
            if accum is not None:
                kw["accum_out"] = accum
            P.op("act", lambda e: e.activation(out, in_, func, **kw), reads, writes)

        def ts(eng, out, in0, s1, s2, op0, op1, reads, writes):
            if op1 is None:
                P.op(eng, lambda e: e.tensor_scalar(out, in0, s1, None, op0), reads, writes)
            else:
                P.op(eng, lambda e: e.tensor_scalar(out, in0, s1, s2, op0, op1), reads, writes)

        def tt(eng, out, in0, in1, op, reads, writes):
            P.op(eng, lambda e: e.tensor_tensor(out, in0, in1, op), reads, writes)

        def stt(eng, out, in0, scalar, in1, op0, op1, reads, writes):
            P.op(eng, lambda e: e.scalar_tensor_tensor(out, in0, scalar, in1, op0, op1), reads, writes)

        def cp(eng, out, in_, reads, writes):
            P.op(eng, lambda e: e.tensor_copy(out, in_), reads, writes)

        def memset(eng, ap, val, writes):
            P.op(eng, lambda e: e.memset(ap, val), (), writes)

        ACC = sb("ACC", [128, NT * D])
        IDENT = sb("IDENT", [128, 128])
        ONES = sb("ONES", [128, 128])
        GATE = sb("GATE", [128, NT * NE])
        COLS = sb("COLS", [128, 64])
        MODT = sb("MODT", [128, 96])
        MX5BC = sb("MX5BC", [128, D])
        EPSC = sb("EPSC", [128, 1])

        def acck(t):
            return "acc%d" % t

        A1X, A1C, A2C, LBT, OMLT, GNC = 0, 8, 16, 24, 32, 40

        def col(base, i):
            return COLS[:, base + i:base + i + 1]

        def modcol(j, which):
            return MODT[:, j * 2 + which:j * 2 + which + 1]


        with ExitStack() as s1:
            XNT = sb("XNT", [128, 8 * 2304], BF16, stack=s1)
            MX2BC = sb("MX2BC", [128, D], stack=s1)
            CM = sb("CM", [128, 898], stack=s1)
            LBBC = sb("LBBC", [128, 1024], stack=s1)
            OMLBC = sb("OMLBC", [128, 1024], stack=s1)

            def xk(t):
                return "xnt%d" % t

            def xnt(kc, t0, n):
                return XNT[:, kc * 2304 + t0:kc * 2304 + t0 + n]

            with ExitStack() as sa:
                G1 = "G:p1"
                G0 = "G:p0"
                CT = sb("CT", [128, 16], stack=sa)
                SC = sb("SC", [128, 16], stack=sa)
                ADAB = sb("ADAB", [128, 48], stack=sa)
                N1G = sb("N1G", [128, 8], stack=sa)
                N2G = sb("N2G", [128, 8], stack=sa)
                LBTR = sb("LBTR", [128, 16], stack=sa)
                TMPC = sb("TMPC", [128, 16], stack=sa)
                DG = [sb("DG%d" % i, [128, 128], stack=sa) for i in range(2)]
                AW = [sb("AW%d" % i, [128, 8 * 512], stack=sa) for i in range(2)]
                dma("sp", IDENT[:], ident_d, (), ["ident"], G0)
                dma("sp", ONES[:], ones_d, (), ["ones"], G0)
                dma("sp", CT[:], cT_d, (), ["ct"], G0)
                dma("sp", ADAB[:], adab_d, (), ["adab"], G0)
                dma("sp", N1G[:], n1g_d, (), ["n1g"], G0)
                dma("sp", N2G[:], n2g_d, (), ["n2g"], G0)
                dma("sp", LBTR[:], lbT_d, (), ["lbtr"], G0)
                dma("sp", COLS[:, GNC:GNC + 1], gn_d, (), ["gnc"], G0)
                memset("dve", EPSC[:], EPS, ["epsc"])
                act(SC[:], CT[:], AF.Silu, ["ct"], ["sc"])
                adaw_v = adaw_d.rearrange("(kc p) n -> p kc n", p=128)
                pm, pmk = nps()
                for j in range(12):
                    s = j % 2
                    dma("sp", AW[s][:].rearrange("p (kc n) -> p kc n", kc=8), adaw_v[:, :, j * 512:(j + 1) * 512],
                        (), ["aw%d" % s], "aw%d" % s)
                    for oc in range(4):
                        gi = j * 4 + oc
                        for kc in range(8):
                            mm(pm[:, gi * 2:gi * 2 + 2], AW[s][:, kc * 512 + oc * 128:kc * 512 + (oc + 1) * 128],
                               SC[:, kc * 2:kc * 2 + 2], kc == 0, kc == 7, ["aw%d" % s, "sc"], [pmk])
                pm3 = pm[:, 0:96].rearrange("p (c t) -> p c t", t=2)
                md3 = MODT[:, 0:96].rearrange("p (c t) -> p c t", t=2)
                for w in range(2):
                    tt("dve", md3[:, :, w], pm3[:, :, w], ADAB[:], ALU.add, [pmk, "adab"], ["modt"])
                mdx = MODT[:, 0:96].rearrange("p (c t) -> p c t", t=2)
                for (base, j0, w, g, gk) in ((A1X, 8, 0, N1G, "n1g"), (A1C, 8, 1, N1G, "n1g"), (A2C, 32, 0, N2G, "n2g")):
                    ts("dve", TMPC[:, 0:8], mdx[:, j0:j0 + 8, w], 1.0, None, ALU.add, None, ["modt"], ["tmpc"])
                    tt("dve", COLS[:, base:base + 8], TMPC[:, 0:8], g[:], ALU.mult, ["tmpc", gk], ["cols"])
                tt("dve", TMPC[:, 0:8], LBTR[:, 0:8], LBTR[:, 8:16], ALU.subtract, ["lbtr"], ["tmpc"])
                act(COLS[:, LBT:LBT + 8], TMPC[:, 0:8], AF.Sigmoid, ["tmpc"], ["cols"])
                ts("dve", COLS[:, OMLT:OMLT + 8], COLS[:, LBT:LBT + 8], -1.0, 1.0, ALU.mult, ALU.add, ["cols"], ["cols"])
                for c in range(8):
                    if c % 4 == 0:
                        pb, pbk = nps()
                    dg = DG[c % 2]
                    ts("dve", dg[:], IDENT[:], modcol(40 + c, 0), None, ALU.mult, None, ["ident", "modt"], ["dg%d" % (c % 2)])
                    mm(pb[:, (c % 4) * 128:(c % 4 + 1) * 128], ONES[:], dg[:], True, True, ["ones", "dg%d" % (c % 2)], [pbk])
                    if c % 4 == 3:
                        cp("dve", MX5BC[:, (c - 3) * 128:(c + 1) * 128], pb[:], [pbk], ["mx5bc"])
                LBR = sb("LBR", [128, 2048], stack=sa)
                POS = [sb("POS%d" % i, [128, D], stack=sa) for i in range(2)]
                XC = [sb("XC%d" % i, [128, D], stack=sa) for i in range(2)]
                YT = [sb("YT%d" % i, [128, D], stack=sa) for i in range(2)]
                SQJ = sb("SQJ", [128, D], BF16, stack=sa)
                SS = sb("SS", [128, NTA], stack=sa)
                RSTD = sb("RSTD", [128, NTA], stack=sa)
                DG2 = [sb("DGb%d" % i, [128, 128], stack=sa) for i in range(2)]
                dma("sp", CM[:], cm_d, (), ["cm"], G1)
                dma("sp", LBR[:], lbrow_d.partition_broadcast(128), (), ["lbr"], G1)
                for t in range(NT):
                    dma("actq", ACC[:, t * D:(t + 1) * D], x_d[t * 128:(t + 1) * 128, :], (), [acck(t)], "xin%d" % t)
                    dma("actq", POS[t % 2][:], pos_d[t * 128:(t + 1) * 128, :], (), ["pos%d" % (t % 2)], "pos%d" % (t % 2))
                    tt("pool", ACC[:, t * D:(t + 1) * D], ACC[:, t * D:(t + 1) * D], POS[t % 2][:], ALU.add,
                       [acck(t), "pos%d" % (t % 2)], [acck(t)])
                    act(SQJ[:], ACC[:, t * D:(t + 1) * D], AF.Square, [acck(t)], ["sqj", "ss"], accum=SS[:, t:t + 1])
                for i in range(2):
                    dma("actq", XC[i][:], ctx_d[i * 128:(i + 1) * 128, :], (), ["xc%d" % i], "xc%d" % i)
                    act(SQJ[:], XC[i][:], AF.Square, ["xc%d" % i], ["sqj", "ss"], accum=SS[:, NT + i:NT + i + 1])
                tt("dve", LBBC[:], LBR[:, 0:1024], LBR[:, 1024:2048], ALU.subtract, ["lbr"], ["lbbc"])
                act(LBBC[:], LBBC[:], AF.Sigmoid, ["lbbc"], ["lbbc"])
                ts("dve", OMLBC[:], LBBC[:], -1.0, 1.0, ALU.mult, ALU.add, ["lbbc"], ["omlbc"])
                for c in range(8):
                    if c % 4 == 0:
                        pb, pbk = nps()
                    dg = DG2[c % 2]
                    ts("dve", dg[:], IDENT[:], modcol(16 + c, 0), None, ALU.mult, None, ["ident", "modt"], ["dgb%d" % (c % 2)])
                    mm(pb[:, (c % 4) * 128:(c % 4 + 1) * 128], ONES[:], dg[:], True, True, ["ones", "dgb%d" % (c % 2)], [pbk])
                    if c % 4 == 3:
                        cp("dve", MX2BC[:, (c - 3) * 128:(c + 1) * 128], pb[:], [pbk], ["mx2bc"])
                act(RSTD[:], SS[:], AF.Sqrt, ["ss", "epsc"], ["rstd"], bias=EPSC[:, 0:1], scale=1.0 / D)
                P.op("dve", lambda e: e.reciprocal(RSTD[:], RSTD[:]), ["rstd"], ["rstd"])
                for t in range(NTA):
                    lat = t < NT
                    src = ACC[:, t * D:(t + 1) * D] if lat else XC[t - NT][:]
                    srck = acck(t) if lat else "xc%d" % (t - NT)
                    y = YT[t % 2]
                    yk = "yt%d" % (t % 2)
                    act(y[:], src, AF.Copy, [srck, "rstd"], [yk], scale=RSTD[:, t:t + 1])
                    abase = A1X if lat else A1C
                    w = 0 if lat else 1
                    for half in range(2):
                        pt, ptk = nps()
                        for c4 in range(4):
                            c = half * 4 + c4
                            tr(pt[:, c4 * 128:(c4 + 1) * 128], y[:, c * 128:(c + 1) * 128], [yk], [ptk])
                        for c4 in range(4):
                            c = half * 4 + c4
                            if half == 0:
                                ts("dve", xnt(c, t * 128, 128), pt[:, c4 * 128:(c4 + 1) * 128], col(abase, c), modcol(c, w),
                                   ALU.mult, ALU.add, [ptk, "cols", "modt"], [xk(t)])
                            else:
                                act(xnt(c, t * 128, 128), pt[:, c4 * 128:(c4 + 1) * 128], AF.Identity,
                                    [ptk, "cols", "modt"], [xk(t)], bias=modcol(c, w), scale=col(abase, c))
                P.emit()

            win_v = win_d.rearrange("(kc p) n -> p kc n", p=128)
            wout_v = wout_d.rearrange("(c p) n -> p c n", p=128)

            with ExitStack() as sn:
              if stop_after != "1A":
                G2 = "G:p2"
                WINF = sb("WINF", [128, 8 * 512], BF16, stack=sn)
                FW = sb("FW", [128, 512], stack=sn)
                DFTC = sb("DFTC", [128, 256], stack=sn)
                CSW = sb("CSW", [128, 4 * 256], BF16, stack=sn)
                WOS = [sb("WOS%d" % i, [128, D], stack=sn) for i in range(1)]
                WOUTF = sb("WOUTF", [128, 4 * D], BF16, stack=sn)
                UT = [sb("UT%d" % i, [128, L], BF16, stack=sn) for i in range(1)]
                AB = sb("AB", [128, 16 * 4 * 256], BF16, stack=sn)
                DF = [sb("DF%d" % i, [128, 4096], BF16, stack=sn) for i in range(2)]
                FXT = sb("FXT", [128, 4 * 512], BF16, stack=sn)
                dma("sp", FW[:], fw_d, (), ["fw"], G2)
                dma("sp", DFTC[:], dftc_d, (), ["dftc"], G2)
                dma("poolq", WINF[:].rearrange("p (kc n) -> p kc n", kc=8), win_v[:, :, 2560:3072], (), ["winf"], "winf")
                for g in range(4):
                    dma("sp", WOS[0][:], wout_v[:, 4 + g, :], (), ["wos0"], "wos0")
                    tt("dve", WOUTF[:, g * D:(g + 1) * D], WOS[0][:], MX2BC[:], ALU.mult,
                       ["wos0", "mx2bc"], ["woutf"])
                for g in range(4):
                    pc, pck = nps()
                    mm(pc[:, 0:128], DFTC[:, 0:128], FW[:, g * 128:(g + 1) * 128], True, True, ["dftc", "fw"], [pck])
                    mm(pc[:, 128:256], DFTC[:, 128:256], FW[:, g * 128:(g + 1) * 128], True, True, ["dftc", "fw"], [pck])
                    cp("dve", CSW[:, g * 256:(g + 1) * 256], pc[:, 0:256], [pck], ["csw"])
                for g in range(4):
                    u = UT[0]
                    uk = "ut0"
                    for t4 in range(4):
                        pu, puk = nps()
                        for kc in range(8):
                            mm(pu[:], WINF[:, kc * 512 + g * 128:kc * 512 + (g + 1) * 128], xnt(kc, t4 * 512, 512),
                               kc == 0, kc == 7, ["winf"] + [xk(t4 * 4 + i) for i in range(4)], [puk])
                        act(u[:, t4 * 512:(t4 + 1) * 512], pu[:], AF.Copy, [puk], [uk])
                    for j2 in range(8):
                        pa, pak = nps()
                        for jj in range(2):
                            j = j2 * 2 + jj
                            mm(pa[:, jj * 256:(jj + 1) * 256], u[:, j * 128:(j + 1) * 128], CSW[:, g * 256:(g + 1) * 256],
                               True, True, [uk, "csw"], [pak])
                        for jj in range(2):
                            j = j2 * 2 + jj
                            o_ap = AB[:, (j * 4 + g) * 256:(j * 4 + g + 1) * 256]
                            if j2 % 2 == 0:
                                cp("dve", o_ap, pa[:, jj * 256:(jj + 1) * 256], [pak], ["ab"])
                            else:
                                act(o_ap, pa[:, jj * 256:(jj + 1) * 256], AF.Copy, [pak], ["ab"])
                pcs = 0
                for lt in range(4):
                    banks = [nps() for _ in range(4)]
                    for cs in range(2):
                        for jh in range(2):
                            sl = pcs % 2
                            dma("sp", DF[sl][:], dftl_d[lt * 4 + cs * 2 + jh], (), ["df%d" % sl], "df%d" % sl)
                            pcs += 1
                            for g in range(4):
                                pg, pgk = banks[g]
                                for j8 in range(8):
                                    j = jh * 8 + j8
                                    first = (cs == 0 and jh == 0 and j8 == 0)
                                    last = (cs == 1 and jh == 1 and j8 == 7)
                                    mm(pg[:], AB[:, (j * 4 + g) * 256 + cs * 128:(j * 4 + g) * 256 + (cs + 1) * 128],
                                       DF[sl][:, j8 * 512:(j8 + 1) * 512], first, last, ["ab", "df%d" % sl], [pgk])
                    for g in range(4):
                        pg, pgk = banks[g]
                        if g % 2 == 0:
                            act(FXT[:, g * 512:(g + 1) * 512], pg[:], AF.Copy, [pgk], ["fxt"])
                        else:
                            cp("dve", FXT[:, g * 512:(g + 1) * 512], pg[:], [pgk], ["fxt"])
                    for tk in range(4):
                        t = lt * 4 + tk
                        for dh in range(2):
                            po, pok = nps()
                            for g in range(4):
                                mm(po[:], FXT[:, g * 512 + tk * 128:g * 512 + (tk + 1) * 128],
                                   WOUTF[:, g * D + dh * 512:g * D + (dh + 1) * 512], g == 0, g == 3, ["fxt", "woutf"], [pok])
                            a_ap = ACC[:, t * D + dh * 512:t * D + (dh + 1) * 512]
                            tt("dve", a_ap, po[:], a_ap, ALU.add, [pok, acck(t)], [acck(t)])
                P.emit()


            with ExitStack() as sh:
              if stop_after not in ("1A", "1N"):
                OTACC = sb("OTACC", [128, 2 * L], stack=sh)
                WQ = sb("WQ", [128, 8 * 256], BF16, stack=sh)
                WV = sb("WV", [128, 8 * 256], BF16, stack=sh)
                WG = sb("WG", [128, 8 * 256], BF16, stack=sh)
                WZ = [sb("WZ%d" % i, [128, 8 * 256], BF16, stack=sh) for i in range(2)]
                WOS2 = sb("WOSb", [128, D], stack=sh)
                WOUTH = sb("WOUTH", [128, 2 * D], BF16, stack=sh)

                def pair(name, dt=F32, w=256):
                    return [sb("%s_%d" % (name, i), [128, w], dt, stack=sh) for i in range(2)]
                T1 = pair("T1"); LOGF = pair("LOGF"); KTM = pair("KTM"); T2 = pair("T2"); KT = pair("KT")
                EPI = pair("EPI", F32, 512); EM = pair("EM"); ERD = pair("ERD", F32, 260)
                VBF = [sb("VBF_%d" % i, [128, 256], BF16, stack=sh) for i in range(3)]; QI = pair("QI", BF16); QE = pair("QE", BF16); KI = pair("KI", BF16)
                KP0 = pair("KP0", BF16); KP1 = pair("KP1", BF16); ATM = pair("ATM", BF16)
                S32s = [sb("S32_%d" % d_, [128, 256], stack=sh) for d_ in range(2)]
                SBFs = [[sb("SBF%d_%d" % (d_, i), [128, 256], BF16, stack=sh) for i in range(2)] for d_ in range(2)]
                OO = sb("OO", [128, 256], stack=sh)
                SQ = sb("SQ", [128, 256], stack=sh)
                RS = sb("RS", [128, 256], stack=sh)
                SG = sb("SG", [128, 256], stack=sh)
                HXB = sb("HXB", [128, 256], BF16, stack=sh)
                MC = [CM[:, 0:128], CM[:, 384:512]]
                MI = [CM[:, 128:256], CM[:, 512:640]]
                M2 = [CM[:, 256:384], CM[:, 640:768]]
                SEL = CM[:, 768:770]
                MASK = [CM[:, 770:834], CM[:, 834:898]]
                for p in range(2):
                    memset("dve", ATM[p][:], 0.0, ["atm%d" % p])
                    memset("dve", KP0[p][:], 0.0, ["kp%d" % p])
                    memset("dve", KP1[p][:], 0.0, ["kp%d" % p])

                from collections import deque
                freeps = deque(range(8))

                def aps():
                    i = freeps.popleft()
                    return PS[i], "ps%d" % i

                def rps(key):
                    freeps.append(int(key[2:]))

                def com(it):
                    hp, dr, t, p = it["hp"], it["dr"], it["t"], it["p"]
                    return hp, dr, t, p, t < NT, t * 128, [xk(t)], "%d" % p, "vbf%d" % it["p3"], VBF[it["p3"]]

                def F1a(it):
                    hp, dr, t, p, lat, tok0, xr, P_, vk, vb = com(it)
                    wz, wzk = WZ[dr], "wz%d" % dr
                    lbo = dr * 512 + hp * 256
                    pzv, pzvk = aps()
                    for kc in range(8):
                        mm(pzv[:, 0:256], xnt(kc, tok0, 128), wz[:, kc * 256:(kc + 1) * 256], kc == 0, kc == 7,
                           xr + [wzk], [pzvk])
                    for kc in range(8):
                        mm(pzv[:, 256:512], xnt(kc, tok0, 128), WV[:, kc * 256:(kc + 1) * 256], kc == 0, kc == 7,
                           xr + ["wv"], [pzvk])
                    act(T1[p][:], pzv[:, 0:256], AF.Sigmoid, [pzvk], ["t1" + P_])
                    act(vb[:], pzv[:, 256:512], AF.Copy, [pzvk], [vk])
                    rps(pzvk)
                    tt("dve", T1[p][:], T1[p][:], OMLBC[:, lbo:lbo + 256], ALU.mult, ["t1" + P_, "omlbc"], ["t1" + P_])
                    tt("dve", T1[p][:], T1[p][:], LBBC[:, lbo:lbo + 256], ALU.add, ["t1" + P_, "lbbc"], ["t1" + P_])
                    act(LOGF[p][:], T1[p][:], AF.Ln, ["t1" + P_], ["logf" + P_])
                    ts("dve", KTM[p][:], T1[p][:], -1.0, 1.0, ALU.mult, ALU.add, ["t1" + P_], ["ktm" + P_])

                def F1b(it):
                    hp, dr, t, p, lat, tok0, xr, P_, vk, vb = com(it)
                    if not lat:
                        return
                    wz, wzk = WZ[dr], "wz%d" % dr
                    pzq, pzqk = aps()
                    for i in range(2):
                        for kc in range(8):
                            mm(pzq[:, i * 128:(i + 1) * 128], wz[:, kc * 256 + i * 128:kc * 256 + (i + 1) * 128],
                               xnt(kc, tok0, 128), kc == 0, kc == 7, xr + [wzk], [pzqk])
                    for i in range(2):
                        for kc in range(8):
                            mm(pzq[:, 256 + i * 128:256 + (i + 1) * 128], WQ[:, kc * 256 + i * 128:kc * 256 + (i + 1) * 128],
                               xnt(kc, tok0, 128), kc == 0, kc == 7, xr + ["wq"], [pzqk])
                    act(T2[p][:], pzq[:, 0:256], AF.Sigmoid, [pzqk], ["t2" + P_], scale=-1.0)
                    for i in range(2):
                        ts("dve", KT[p][:, i * 128:(i + 1) * 128], T2[p][:, i * 128:(i + 1) * 128],
                           col(OMLT, dr * 4 + hp * 2 + i), None, ALU.mult, None, ["t2" + P_, "cols"], ["kt" + P_])
                    it["pzq"] = (pzq, pzqk)

                def F2a(it):
                    hp, dr, t, p, lat, tok0, xr, P_, vk, vb = com(it)
                    pr, prk = aps()
                    mm(pr[:, 0:256], M2[dr], LOGF[p][:], True, True, ["cm", "logf" + P_], [prk])
                    for i in range(2):
                        mm(pr[:, 256 + i * 2:256 + i * 2 + 2], LOGF[p][:, i * 128:(i + 1) * 128], SEL, True, True,
                           ["cm", "logf" + P_], [prk])
                    if lat:
                        pcm, pcmk = aps()
                        for i in range(2):
                            mm(pcm[:, i * 128:(i + 1) * 128], LOGF[p][:, i * 128:(i + 1) * 128], MC[dr], True, True,
                               ["cm", "logf" + P_], [pcmk])
                        for i in range(2):
                            mm(pcm[:, 256 + i * 128:256 + (i + 1) * 128], LOGF[p][:, i * 128:(i + 1) * 128], MI[dr], True, True,
                               ["cm", "logf" + P_], [pcmk])
                    act(ERD[p][:], pr[:, 0:260], AF.Exp, [prk], ["er" + P_, "dec" + P_])
                    rps(prk)
                    if lat:
                        act(EPI[p][:], pcm[:, 0:512], AF.Exp, [pcmk], ["ep" + P_, "ei" + P_])
                        rps(pcmk)
                        P.op("dve", lambda e: e.reciprocal(EM[p][:], EPI[p][:, 0:256]), ["ep" + P_], ["em" + P_])

                def F2b(it):
                    hp, dr, t, p, lat, tok0, xr, P_, vk, vb = com(it)
                    tt("dve", KP0[p][0:64, :], KTM[p][0:64, :], ERD[p][0:64, 0:256], ALU.mult, ["ktm" + P_, "er" + P_], ["kp" + P_])
                    tt("dve", KP1[p][64:128, :], KTM[p][64:128, :], ERD[p][64:128, 0:256], ALU.mult, ["ktm" + P_, "er" + P_], ["kp" + P_])
                    pu, puk = aps()
                    for c in range(2):
                        kp = KP0[p] if c == 0 else KP1[p]
                        for i in range(2):
                            mm(pu[:, c * 256 + i * 128:c * 256 + (i + 1) * 128], kp[:, i * 128:(i + 1) * 128],
                               vb[:, i * 128:(i + 1) * 128], True, True, ["kp" + P_, vk], [puk])
                    it["pu"] = (pu, puk)
                    if not lat:
                        return
                    pzq, pzqk = it["pzq"]
                    tt("dve", QI[p][:], pzq[:, 256:512], EPI[p][:, 0:256], ALU.mult, [pzqk, "ep" + P_], ["qi" + P_])
                    tt("dve", KI[p][:], KT[p][:], EM[p][:], ALU.mult, ["kt" + P_, "em" + P_], ["ki" + P_])
                    tt("dve", QE[p][:], pzq[:, 256:512], EPI[p][:, 256:512], ALU.mult, [pzqk, "ei" + P_], ["qe" + P_])
                    rps(pzqk)
                    pa, pak = aps()
                    for i in range(2):
                        mm(pa[:, i * 128:(i + 1) * 128], KI[p][:, i * 128:(i + 1) * 128], QI[p][:, i * 128:(i + 1) * 128],
                           True, True, ["ki" + P_, "qi" + P_], [pak])
                    for i in range(2):
                        for c in range(2):
                            r0, r1 = c * 64, (c + 1) * 64
                            tt("dve", ATM[p][r0:r1, i * 128 + c * 64:i * 128 + (c + 1) * 64],
                               pa[r0:r1, i * 128 + c * 64:i * 128 + (c + 1) * 64], MASK[dr][r0:r1, :], ALU.mult,
                               [pak, "cm"], ["atm" + P_])
                    rps(pak)

                def chunk_step(it, ci):
                    hp, dr, t, p, lat, tok0, xr, P_, vk, vb = com(it)
                    pu, puk = it["pu"]
                    c = ((0, 1) if dr == 0 else (1, 0))[ci]
                    sidx = it["sidx"]
                    S32 = S32s[dr]
                    SBF = SBFs[dr]
                    sk = "s32_%d" % dr
                    if lat:
                        for i in range(2):
                            po, pok = it["pos"][i]
                            mm(po[:, c * 64:(c + 1) * 64], SBF[sidx][:, i * 128:(i + 1) * 128],
                               QE[p][:, i * 128 + c * 64:i * 128 + (c + 1) * 64], False, ci == 1,
                               ["sbf%d_%d" % (dr, sidx), "qe" + P_], [pok])
                    for i in range(2):
                        stt("dve", S32[:, i * 128:(i + 1) * 128], S32[:, i * 128:(i + 1) * 128],
                            ERD[p][:, 256 + i * 2 + c:256 + i * 2 + c + 1], pu[:, c * 256 + i * 128:c * 256 + (i + 1) * 128],
                            ALU.mult, ALU.add, [sk, "dec" + P_, puk], [sk])
                    nidx = 1 - sidx
                    act(SBF[nidx][:], S32[:], AF.Copy, [sk], ["sbf%d_%d" % (dr, nidx)])
                    it["sidx"] = nidx

                def Ba(it):
                    hp, dr, t, p, lat, tok0, xr, P_, vk, vb = com(it)
                    it["sidx"] = 0
                    if lat:
                        it["pos"] = [aps() for _ in range(2)]
                        for i in range(2):
                            po, pok = it["pos"][i]
                            mm(po[:, 0:128], vb[:, i * 128:(i + 1) * 128], ATM[p][:, i * 128:(i + 1) * 128], True, False,
                               [vk, "atm" + P_], [pok])
                    chunk_step(it, 0)

                def Bb(it):
                    hp, dr, t, p, lat, tok0, xr, P_, vk, vb = com(it)
                    chunk_step(it, 1)
                    rps(it["pu"][1])
                    if not lat:
                        return
                    if not it["fin"]:
                        for i in range(2):
                            po, pok = it["pos"][i]
                            cp("dve", OTACC[:, i * L + tok0:i * L + tok0 + 128], po[:, 0:128], [pok], ["otacc%d" % t])
                            rps(pok)
                        return
                    for i in range(2):
                        po, pok = it["pos"][i]
                        tt("dve", OO[:, i * 128:(i + 1) * 128], po[:, 0:128], OTACC[:, i * L + tok0:i * L + tok0 + 128],
                           ALU.add, [pok, "otacc%d" % t], ["oo"])
                        rps(pok)
                    act(SQ[:], OO[:], AF.Square, ["oo"], ["sq"])

                def Bc(it):
                    hp, dr, t, p, lat, tok0, xr, P_, vk, vb = com(it)
                    if not lat or not it["fin"]:
                        return
                    pss, pssk = aps()
                    for i in range(2):
                        for kc in range(8):
                            mm(pss[:, 256 + i * 128:256 + (i + 1) * 128], WG[:, kc * 256 + i * 128:kc * 256 + (i + 1) * 128],
                               xnt(kc, tok0, 128), kc == 0, kc == 7, xr + ["wg"], [pssk])
                    mm(pss[:, 0:256], ONES[:], SQ[:], True, True, ["ones", "sq"], [pssk])
                    act(RS[:], pss[:, 0:256], AF.Sqrt, [pssk, "epsc"], ["rs"], bias=EPSC[:, 0:1], scale=1.0 / 128)
                    act(SG[:], pss[:, 256:512], AF.Sigmoid, [pssk], ["sg"])
                    rps(pssk)
                    P.op("dve", lambda e: e.reciprocal(RS[:], RS[:]), ["rs"], ["rs"])
                    tt("dve", OO[:], OO[:], RS[:], ALU.mult, ["oo", "rs"], ["oo"])
                    stt("dve", HXB[:], OO[:], COLS[:, GNC:GNC + 1], SG[:], ALU.mult, ALU.mult, ["oo", "gnc", "sg"], ["hxb"])
                    for dh in range(2):
                        px, pxk = aps()
                        for i in range(2):
                            mm(px[:], HXB[:, i * 128:(i + 1) * 128], WOUTH[:, i * D + dh * 512:i * D + (dh + 1) * 512],
                               i == 0, i == 1, ["hxb", "wouth"], [pxk])
                        a_ap = ACC[:, t * D + dh * 512:t * D + (dh + 1) * 512]
                        tt("dve", a_ap, px[:], a_ap, ALU.add, [pxk, acck(t)], [acck(t)])
                        rps(pxk)

                itn = 0
                for hp in range(2):
                    c0 = hp * 256

                    def wload(dst, colbase, key):
                        dma("poolq", dst[:].rearrange("p (kc n) -> p kc n", kc=8), win_v[:, :, colbase + c0:colbase + c0 + 256],
                            (), [key], key)
                    wload(WZ[0], 1536, "wz0")
                    wload(WV, 512, "wv")
                    wload(WQ, 0, "wq")
                    wload(WZ[1], 2048, "wz1")
                    wload(WG, 1024, "wg")
                    for i in range(2):
                        dma("sp", WOS2[:], wout_v[:, hp * 2 + i, :], (), ["wosb"], "wosb")
                        tt("pool", WOUTH[:, i * D:(i + 1) * D], WOS2[:], MX2BC[:], ALU.mult, ["wosb", "mx2bc"], ["wouth"])
                    for dr in range(2):
                        memset("dve", S32s[dr][:], 0.0, ["s32_%d" % dr])
                        memset("dve", SBFs[dr][0][:], 0.0, ["sbf%d_0" % dr])
                    ordf = [16, 17] + list(range(16))
                    ordb = [17, 16] + list(range(15, -1, -1))
                    its = []
                    seen = set()
                    for k in range(18):
                        for dr, t in ((0, ordf[k]), (1, ordb[k])):
                            fin = (t in seen)
                            if t < NT:
                                seen.add(t)
                            its.append({"hp": hp, "dr": dr, "t": t, "p": itn % 2, "p3": itn % 3, "cur": 0, "fin": fin})
                            itn += 1
                    ni = len(its)
                    F1a(its[0]); F1b(its[0])
                    F1a(its[1]); F1b(its[1])
                    F2a(its[0]); F2b(its[0])
                    for n in range(ni):
                        n1 = its[n + 1] if n + 1 < ni else None
                        n2 = its[n + 2] if n + 2 < ni else None
                        if n1: F2a(n1)
                        Ba(its[n])
                        if n2: F1a(n2)
                        if n1: F2b(n1)
                        Bb(its[n])
                        if n2: F1b(n2)
                        Bc(its[n])
                P.emit()

        with ExitStack() as s2:
            HMT = sb("HMT", [128, 8 * L], BF16, stack=s2)
            SS2 = sb("SS2", [128, NT], stack=s2)
            RSTD2 = sb("RSTD2", [128, NT], stack=s2)
            B1T = sb("B1T", [128, NE * 16], stack=s2)

            def hk(t):
                return "hmt%d" % t

            if stop_after is None or stop_after in ("2", "3"):
                with ExitStack() as sr:
                    G3 = "G:p3"
                    RW = sb("RW", [128, 256], stack=sr)
                    RBBC = sb("RBBC", [128, NE], stack=sr)
                    B2P = sb("B2P", [NE, D], stack=sr)
                    SQJ2 = sb("SQJ2", [128, D], BF16, stack=sr)
                    Y2 = [sb("Y2_%d" % i, [128, D], stack=sr) for i in range(2)]
                    HM32 = [sb("HM32_%d" % i, [128, D], stack=sr) for i in range(2)]
                    LG = sb("LG", [128, NE], stack=sr)
                    M8 = sb("M8", [128, 8], stack=sr)
                    NEGM = sb("NEGM", [128, 1], stack=sr)
                    EX = sb("EX", [128, NE], stack=sr)
                    MSK = sb("MSK", [128, NE], stack=sr)
                    SUMC = sb("SUMC", [128, 1], stack=sr)
                    GT = [sb("GT%d" % i, [NE, 128], stack=sr) for i in range(2)]
                    dma("sp", RW[:], rw_d, (), ["rw"], G3)
                    dma("sp", RBBC[:], rb_d.partition_broadcast(128), (), ["rbbc"], G3)
                    dma("sp", B2P[:], b2_d, (), ["b2p"], G3)
                    dma("sp", B1T[:], b1_d, (), ["b1t"], G3)
                    tt("dve", B2P[:], B2P[:], MX5BC[0:NE, :], ALU.mult, ["b2p", "mx5bc"], ["b2p"])
                    b13 = B1T[:].rearrange("p (e c) -> p e c", c=16)
                    ts("dve", b13[:, :, 8:16], b13[:, :, 8:16], 1.0, None, ALU.add, None, ["b1t"], ["b1t"])
                    for t in range(NT):
                        act(SQJ2[:], ACC[:, t * D:(t + 1) * D], AF.Square, [acck(t)], ["sqj2", "ss2"], accum=SS2[:, t:t + 1])
                    act(RSTD2[:], SS2[:], AF.Sqrt, ["ss2", "epsc"], ["rstd2"], bias=EPSC[:, 0:1], scale=1.0 / D)
                    P.op("dve", lambda e: e.reciprocal(RSTD2[:], RSTD2[:]), ["rstd2"], ["rstd2"])
                    def stA(t):
                        y = Y2[t % 2]
                        yk = "y2_%d" % (t % 2)
                        h32 = HM32[t % 2]
                        h32k = "hm32_%d" % (t % 2)
                        act(y[:], ACC[:, t * D:(t + 1) * D], AF.Copy, [acck(t), "rstd2"], [yk], scale=RSTD2[:, t:t + 1])
                        for half in range(2):
                            pt, ptk = nps()
                            for c4 in range(4):
                                c = half * 4 + c4
                                tr(pt[:, c4 * 128:(c4 + 1) * 128], y[:, c * 128:(c + 1) * 128], [yk], [ptk])
                            for c4 in range(4):
                                c = half * 4 + c4
                                if half == 0:
                                    ts("dve", h32[:, c * 128:(c + 1) * 128], pt[:, c4 * 128:(c4 + 1) * 128], col(A2C, c),
                                       modcol(24 + c, 0), ALU.mult, ALU.add, [ptk, "cols", "modt"], [h32k])
                                else:
                                    act(h32[:, c * 128:(c + 1) * 128], pt[:, c4 * 128:(c4 + 1) * 128], AF.Identity,
                                        [ptk, "cols", "modt"], [h32k], bias=modcol(24 + c, 0), scale=col(A2C, c))
                        for c in range(8):
                            eng = "pool" if c % 2 == 0 else "act"
                            if eng == "pool":
                                cp("pool", HMT[:, c * L + t * 128:c * L + (t + 1) * 128], h32[:, c * 128:(c + 1) * 128], [h32k], [hk(t)])
                            else:
                                act(HMT[:, c * L + t * 128:c * L + (t + 1) * 128], h32[:, c * 128:(c + 1) * 128], AF.Copy, [h32k], [hk(t)])
                        pl, plk = nps()
                        for c in range(8):
                            mm(pl[:, 0:NE], h32[:, c * 128:(c + 1) * 128], RW[:, c * NE:(c + 1) * NE], c == 0, c == 7,
                               [h32k, "rw"], [plk])
                        return (h32, h32k, pl, plk)
                    def stB(t, st):
                        h32, h32k, pl, plk = st
                        tt("dve", LG[:], pl[:, 0:NE], RBBC[:], ALU.add, [plk, "rbbc"], ["lg"])
                        P.op("dve", lambda e: e.max(out=M8[:], in_=LG[:]), ["lg"], ["m8"])
                        ts("dve", NEGM[:], M8[:, 0:1], -1.0, None, ALU.mult, None, ["m8"], ["negm"])
                        act(EX[:], LG[:], AF.Exp, ["lg", "negm"], ["ex"], bias=NEGM[:, 0:1], scale=1.0)
                        ts("dve", MSK[:], LG[:], M8[:, 3:4], None, ALU.is_ge, None, ["lg", "m8"], ["msk"])
                        tt("dve", EX[:], EX[:], MSK[:], ALU.mult, ["ex", "msk"], ["ex"])
                        P.op("dve", lambda e: e.tensor_reduce(SUMC[:], EX[:], AX.X, ALU.add), ["ex"], ["sumc"])
                        P.op("dve", lambda e: e.reciprocal(SUMC[:], SUMC[:]), ["sumc"], ["sumc"])
                        g_ap = GATE[:, t * NE:(t + 1) * NE]
                        ts("dve", g_ap, EX[:], SUMC[:, 0:1], None, ALU.mult, None, ["ex", "sumc"], ["gate%d" % t])
                        pgt, pgtk = nps()
                        tr(pgt[0:NE, 0:128], g_ap, ["gate%d" % t], [pgtk])
                        gt = GT[t % 2]
                        gtk = "gt%d" % (t % 2)
                        act(gt[:], pgt[0:NE, 0:128], AF.Copy, [pgtk], [gtk])
                        for dh in range(2):
                            pb2, pb2k = nps()
                            mm(pb2[:], gt[:], B2P[:, dh * 512:(dh + 1) * 512], True, True, [gtk, "b2p"], [pb2k])
                            a_ap = ACC[:, t * D + dh * 512:t * D + (dh + 1) * 512]
                            tt("dve", a_ap, pb2[:], a_ap, ALU.add, [pb2k, acck(t)], [acck(t)])
                    stq = {0: stA(0)}
                    for t in range(NT):
                        if t + 1 < NT:
                            stq[t + 1] = stA(t + 1)
                        stB(t, stq.pop(t))
                    P.emit()

            if stop_after is None or stop_after == "3":
                with ExitStack() as sm:
                    HHT = sb("HHT", [128, 8 * L], BF16, stack=sm)
                    W1S = [sb("W1S%d" % i, [128, 2048], stack=sm) for i in range(2)]
                    W1B = [sb("W1B%d" % i, [128, 2048], BF16, stack=sm) for i in range(2)]
                    W2S = [sb("W2S%d" % i, [128, D], stack=sm) for i in range(2)]
                    W2B = sb("W2B", [128, 8 * D], BF16, stack=sm)
                    AG = [sb("AG%d" % i, [128, 512], stack=sm) for i in range(2)]
                    SGM = [sb("SGM%d" % i, [128, 512], stack=sm) for i in range(2)]
                    TL = [sb("TL%d" % i, [128, 512], stack=sm) for i in range(2)]
                    QQ = [sb("QQ%d" % i, [128, 512], stack=sm) for i in range(2)]
                    allh = [hk(t) for t in range(NT)]
                    npc = NE * 8

                    def issue_loads(n):
                        e, fc = divmod(n, 8)
                        s = n % 2
                        dma("sp", W1S[s][:], w1_d[n], (), ["w1s%d" % s], "w1s%d" % s)
                        dma("sp", W2S[s][:], w2_d[n], (), ["w2s%d" % s], "w2s%d" % s)

                    def cast_w1(m):
                        sm = m % 2
                        act(W1B[sm][:], W1S[sm][:], AF.Copy, ["w1s%d" % sm], ["w1b%d" % sm])

                    issue_loads(0)
                    cast_w1(0)
                    ucnt = 0
                    for n in range(npc):
                        e, fc = divmod(n, 8)
                        s = n % 2
                        if n + 1 < npc:
                            issue_loads(n + 1)
                        tt("pool", W2B[:, fc * D:(fc + 1) * D], W2S[s][:], MX5BC[:], ALU.mult,
                           ["w2s%d" % s, "mx5bc"], ["w2b%d" % fc])
                        for t4 in range(4):
                            u = ucnt % 2
                            ucnt += 1
                            hr = [hk(t4 * 4 + i) for i in range(4)]
                            pgl = []
                            for half in range(2):
                                pb, pbk = nps()
                                pgl.append((pb, pbk))
                                for kc in range(8):
                                    mm(pb[:], W1B[s][:, kc * 256 + half * 128:kc * 256 + (half + 1) * 128],
                                       HMT[:, kc * L + t4 * 512:kc * L + (t4 + 1) * 512], kc == 0, kc == 7,
                                       ["w1b%d" % s] + hr, [pbk])
                            (pgp, pgk), (plp, plk) = pgl
                            b1g = B1T[:, e * 16 + fc:e * 16 + fc + 1]
                            b1l = B1T[:, e * 16 + 8 + fc:e * 16 + 8 + fc + 1]
                            ts("dve", AG[u][:], pgp[:], b1g, 7.0, ALU.add, ALU.min, [pgk, "b1t"], ["ag%d" % u])
                            act(SGM[u][:], AG[u][:], AF.Sigmoid, ["ag%d" % u], ["sgm%d" % u], scale=1.702)
                            ts("dve", TL[u][:], plp[:], b1l, 8.0, ALU.add, ALU.min, [plk, "b1t"], ["tl%d" % u])
                            stt("dve", QQ[u][:], TL[u][:], -6.0, AG[u][:], ALU.max, ALU.mult, ["tl%d" % u, "ag%d" % u], ["qq%d" % u])
                            tt("pool", HHT[:, fc * L + t4 * 512:fc * L + (t4 + 1) * 512], QQ[u][:], SGM[u][:], ALU.mult,
                               ["qq%d" % u, "sgm%d" % u], ["hht%d_%d" % (fc, t4)])
                            if t4 == 0 and n + 1 < npc:
                                cast_w1(n + 1)
                        if fc == 7:
                            for tk in range(NT):
                                for dh in range(2):
                                    po, pok = nps()
                                    for f in range(8):
                                        mm(po[:], HHT[:, f * L + tk * 128:f * L + (tk + 1) * 128],
                                           W2B[:, f * D + dh * 512:f * D + (dh + 1) * 512], f == 0, f == 7,
                                           ["hht%d_%d" % (f, tk // 4), "w2b%d" % f], [pok])
                                    a_ap = ACC[:, tk * D + dh * 512:tk * D + (dh + 1) * 512]
                                    stt("dve", a_ap, po[:], GATE[:, tk * NE + e:tk * NE + e + 1], a_ap, ALU.mult, ALU.add,
                                        [pok, "gate%d" % tk, acck(tk)], [acck(tk)])
                    P.emit()

            with ExitStack() as sf:
                G4 = "G:p4"
                FGBC = sb("FGBC", [128, D], stack=sf)
                SQJ3 = sb("SQJ3", [128, D], BF16, stack=sf)
                SS3 = sb("SS3", [128, NT], stack=sf)
                OUTT = [sb("OUTT%d" % i, [128, D], stack=sf) for i in range(2)]
                dma("sp", FGBC[:], fg_d.partition_broadcast(128), (), ["fgbc"], G4)
                final_norm = stop_after is None
                if final_norm:
                    for t in range(NT):
                        act(SQJ3[:], ACC[:, t * D:(t + 1) * D], AF.Square, [acck(t)], ["sqj3", "ss3"], accum=SS3[:, t:t + 1])
                    act(SS3[:], SS3[:], AF.Sqrt, ["ss3", "epsc"], ["ss3"], bias=EPSC[:, 0:1], scale=1.0 / D)
                    P.op("dve", lambda e: e.reciprocal(SS3[:], SS3[:]), ["ss3"], ["ss3"])
                for t in range(NT):
                    o = OUTT[t % 2]
                    ok = "outt%d" % (t % 2)
                    if final_norm:
                        stt("dve", o[:], ACC[:, t * D:(t + 1) * D], SS3[:, t:t + 1], FGBC[:], ALU.mult, ALU.mult,
                            [acck(t), "ss3", "fgbc"], [ok])
                    else:
                        cp("dve", o[:], ACC[:, t * D:(t + 1) * D], [acck(t)], [ok])
                    dma("sp", out_d[t * 128:(t + 1) * 128, :], o[:], [ok], (), "out%d" % (t % 2))
                P.out_keys = ["out0", "out1"]
                P.emit(final_wait_out=True)
    return nc


def _prep_inputs(x, c, ctx, c_ctx, ada_w, ada_b, norm1_g, w_in, hgrn_lb, hgrn_gnorm, fnet_w, w_out,
                 norm2_g, router_w, router_b, moe_w1, moe_b1, moe_w2, moe_b2, final_g):
    f = lambda a: np.ascontiguousarray(np.asarray(a, dtype=np.float32))
    cs = _get_consts()
    shared = {}
    shared["pos"] = cs["pos"]
    shared["ada_w"] = f(ada_w[0])
    shared["ada_bT"] = f(np.asarray(ada_b[0]).reshape(48, 128).T)
    shared["n1gT"] = f(np.asarray(norm1_g[0]).reshape(8, 128).T)
    shared["n2gT"] = f(np.asarray(norm2_g[0]).reshape(8, 128).T)
    shared["w_in"] = f(w_in[0])
    lb = np.asarray(hgrn_lb, dtype=np.float32)
    shared["lbT"] = f(lb.reshape(2, 2, 4, 128).transpose(3, 0, 1, 2).reshape(128, 16))
    shared["lbrow"] = f(lb.reshape(1, 2048))
    shared["gn"] = f(np.asarray(hgrn_gnorm[0]).reshape(128, 1))
    shared["fnet_w"] = f(np.asarray(fnet_w[0]).transpose(1, 0, 2).reshape(128, 512))
    shared["w_out"] = f(w_out[0])
    shared["rw"] = f(np.asarray(router_w[0]).reshape(8, 128, NE).transpose(1, 0, 2).reshape(128, 256))
    shared["rb"] = f(np.asarray(router_b[0]).reshape(1, NE))
    w1 = np.asarray(moe_w1[0], dtype=np.float32)
    w1r = w1.reshape(NE, 8, 128, 2, 8, 128).transpose(0, 4, 2, 1, 3, 5)
    shared["w1r"] = np.ascontiguousarray(w1r).reshape(NE * 8, 128, 2048)
    shared["b1T"] = f(np.asarray(moe_b1[0]).reshape(NE, 16, 128).transpose(2, 0, 1).reshape(128, NE * 16))
    shared["w2"] = f(moe_w2[0]).reshape(NE * 8, 128, D)
    shared["b2"] = f(moe_b2[0])
    shared["fg"] = f(np.asarray(final_g).reshape(1, D))
    shared["ident"] = cs["ident"]
    shared["ones"] = cs["ones"]
    shared["cm"] = cs["cm"]
    shared["dftc"] = cs["dftc"]
    shared["dftl"] = cs["dftl"]
    x = np.asarray(x, dtype=np.float32)
    c = np.asarray(c, dtype=np.float32)
    ctx = np.asarray(ctx, dtype=np.float32)
    c_ctx = np.asarray(c_ctx, dtype=np.float32)
    in_maps = []
    for b in range(8):
        m = dict(shared)
        m["x"] = np.ascontiguousarray(x[b])
        m["ctx"] = np.ascontiguousarray(ctx[b])
        cc = np.stack([c[b].reshape(8, 128).T, c_ctx.reshape(8, 128).T], axis=-1)
        m["cT"] = np.ascontiguousarray(cc.reshape(128, 16)).astype(np.float32)
        in_maps.append(m)
    return in_maps


_NC_CACHE = {}


def kernel(**inputs):
    in_maps = _prep_inputs(**inputs)
    if "nc" not in _NC_CACHE:
        _NC_CACHE["nc"] = build()
    nc = _NC_CACHE["nc"]
    res = run_bass_kernel_spmd(nc, in_maps, core_ids=list(range(8)))
    out = np.stack([np.asarray(r["out"], dtype=np.float32) for r in res.results], axis=0)
    return out.reshape(8, L, D)
```

```python
import numpy as np
import ml_dtypes
from contextlib import ExitStack
import concourse.bass as bass
import concourse.mybir as mybir
from concourse.bass_utils import run_bass_kernel_spmd

F32 = mybir.dt.float32
BF16 = mybir.dt.bfloat16
AF = mybir.ActivationFunctionType
ALU = mybir.AluOpType
AX = mybir.AxisListType

D = 1024
L = 2048
LC = 256
NT = 16
NTA = 18
NE = 32
EPS = 1e-6

STREAM_OF = {"pe": "pe", "act": "act", "dve": "dve", "pool": "pool",
             "sp": "sp", "poolq": "pool", "actq": "act"}


class Op:
    __slots__ = ("eng", "fn", "is_dma", "semkey", "phase", "seq", "needs_inc",
                 "inc_val", "waits", "dma_val", "stream")


class Prog:
    def __init__(self, nc, sem_alloc):
        self.nc = nc
        self.sem_alloc = sem_alloc
        self.phase = 0
        self.ops = []
        self.last_w = {}
        self.readers = {}
        self.eng_sem = {}
        self.eng_cnt = {s: 0 for s in ("pe", "act", "dve", "pool", "sp")}
        self.dma_sem = {}
        self.dma_cnt = {}
        self.waited = {}
        self.seqc = {s: 0 for s in ("pe", "act", "dve", "pool", "sp")}
        self.out_keys = []
        self.nops = 0

    def _sem_for_stream(self, s):
        if s not in self.eng_sem:
            self.eng_sem[s] = self.sem_alloc("e_" + s)
        return self.eng_sem[s]

    def _sem_for_key(self, k):
        if k not in self.dma_sem:
            self.dma_sem[k] = self.sem_alloc("d_" + str(k).replace(":", "_"))
            self.dma_cnt[k] = 0
        return self.dma_sem[k]

    def op(self, eng, fn, reads=(), writes=(), semkey=None):
        o = Op()
        o.eng = eng
        o.fn = fn
        o.is_dma = semkey is not None
        o.semkey = semkey
        o.phase = self.phase
        o.stream = STREAM_OF[eng]
        self.seqc[o.stream] += 1
        o.seq = self.seqc[o.stream]
        o.needs_inc = False
        o.inc_val = None
        o.waits = []
        o.dma_val = None
        if o.is_dma:
            self._sem_for_key(semkey)
            self.dma_cnt[semkey] += 16
            o.dma_val = self.dma_cnt[semkey]
        deps = []
        for k in reads:
            w = self.last_w.get(k)
            if w is not None:
                deps.append((w, True))
        for k in writes:
            w = self.last_w.get(k)
            if w is not None:
                deps.append((w, False))
            deps.extend((r, False) for r in self.readers.get(k, ()))
        best = {}
        seen_dma = set()
        for d, raw in deps:
            if d is o:
                continue
            if d.is_dma:
                if id(d) not in seen_dma:
                    seen_dma.add(id(d))
                    o.waits.append(("dma", d))
            else:
                if d.phase != self.phase:
                    continue
                if d.stream == o.stream and not o.is_dma:
                    if not raw or o.stream == "pe":
                        continue
                b = best.get(d.stream)
                if b is None or d.seq > b.seq:
                    best[d.stream] = d
        for s, d in best.items():
            o.waits.append(("eng", d))
        for k in reads:
            self.readers.setdefault(k, []).append(o)
        for k in writes:
            self.last_w[k] = o
            self.readers[k] = []
        self.ops.append(o)
        self.nops += 1
        return o

    def emit(self, final_wait_out=False):
        nc = self.nc
        ops = self.ops
        for o in ops:
            for kind, d in o.waits:
                if kind == "eng":
                    d.needs_inc = True
        for o in ops:
            if not o.is_dma and o.needs_inc:
                self._sem_for_stream(o.stream)
                self.eng_cnt[o.stream] += 1
                o.inc_val = self.eng_cnt[o.stream]
        by_stream = {s: [] for s in ("pe", "act", "dve", "pool", "sp")}
        for o in ops:
            by_stream[o.stream].append(o)
        waited = self.waited
        prog = self

        def run(stream, e):
            for o in by_stream[stream]:
                for kind, d in o.waits:
                    if kind == "dma":
                        sem = prog.dma_sem[d.semkey]
                        nm = ("d", d.semkey)
                        if str(d.semkey).startswith("G:"):
                            val = prog.dma_cnt[d.semkey]
                        else:
                            val = d.dma_val
                    else:
                        sem = prog.eng_sem[d.stream]
                        nm = ("e", d.stream)
                        val = d.inc_val
                    if waited.get((stream, nm), 0) >= val:
                        continue
                    waited[(stream, nm)] = val
                    e.wait_ge(sem, val)
                ins = o.fn(e)
                if o.is_dma:
                    ins.then_inc(prog.dma_sem[o.semkey], 16)
                elif o.needs_inc:
                    ins.then_inc(prog.eng_sem[o.stream], 1)
            if final_wait_out and stream == "sp":
                for k in prog.out_keys:
                    e.wait_ge(prog.dma_sem[k], prog.dma_cnt[k])

        with nc.Block() as block:
            if by_stream["sp"] or final_wait_out:
                @block.sync
                def _(e):
                    run("sp", e)
            if by_stream["pe"]:
                @block.tensor
                def _(e):
                    run("pe", e)
            if by_stream["act"]:
                @block.scalar
                def _(e):
                    run("act", e)
            if by_stream["dve"]:
                @block.vector
                def _(e):
                    run("dve", e)
            if by_stream["pool"]:
                @block.gpsimd
                def _(e):
                    run("pool", e)
        self.ops = []
        self.phase += 1


def _pos_table():
    quarter = D // 4
    omega = (1.0 / (np.float32(10000.0) ** (np.arange(quarter, dtype=np.float32) / np.float32(quarter)))).astype(np.float32)

    def axis_emb(n):
        ang = np.arange(n, dtype=np.float32)[:, None] * omega[None, :]
        return np.concatenate([np.sin(ang), np.cos(ang)], axis=-1).astype(np.float32)

    rows, cols = L // 64, 64
    er = np.broadcast_to(axis_emb(rows)[:, None, :], (rows, cols, D // 2))
    ec = np.broadcast_to(axis_emb(cols)[None, :, :], (rows, cols, D // 2))
    return np.ascontiguousarray(np.concatenate([er, ec], axis=-1).reshape(rows * cols, D)).astype(np.float32)


def _consts():
    c = {}
    c["pos"] = _pos_table()
    c["ident"] = np.eye(128, dtype=np.float32)
    c["ones"] = np.ones((128, 128), dtype=np.float32)
    s = np.arange(128)
    same = (s[:, None] // 64) == (s[None, :] // 64)
    si = (s % 64)[:, None]
    ti = (s % 64)[None, :]
    mi0 = same & (si <= ti)
    mc0 = same * ((si <= ti).astype(np.float32) - (si <= 31).astype(np.float32))
    m20 = same & (si > ti)
    mi1 = same & (si >= ti)
    mc1 = same * ((si >= ti).astype(np.float32) - (si >= 32).astype(np.float32))
    m21 = same & (si < ti)
    sel = np.zeros((128, 2), np.float32)
    sel[:64, 0] = 1
    sel[64:, 1] = 1
    p_in = (s % 64)[:, None]
    t_in = np.arange(64)[None, :]
    mask0 = (p_in <= t_in).astype(np.float32)
    mask1 = (p_in >= t_in).astype(np.float32)
    cm = np.concatenate([mc0, mi0, m20, mc1, mi1, m21, sel, mask0, mask1], axis=1).astype(np.float32)
    c["cm"] = np.ascontiguousarray(cm)
    k = np.arange(128, dtype=np.float64)
    ang = 2.0 * np.pi * np.outer(k, k) / 128.0
    c["dftc"] = np.ascontiguousarray(np.concatenate([np.cos(ang) / 512.0, -np.sin(ang) / 512.0], axis=1)).astype(np.float32)
    kk = np.arange(L, dtype=np.int64)
    ph = (np.outer(kk, kk) % L).astype(np.float64) * (2.0 * np.pi / L)
    mats = [np.cos(ph), np.sin(ph)]
    pieces = np.empty((4, 2, 2, 128, 8, 512), dtype=ml_dtypes.bfloat16)
    for cs in range(2):
        m = mats[cs].reshape(2, 8, 128, 4, 512)
        pieces[:, cs] = np.transpose(m, (3, 0, 2, 1, 4)).astype(ml_dtypes.bfloat16)
    c["dftl"] = np.ascontiguousarray(pieces.reshape(16, 128, 4096))
    return c


_CONSTS = None


def _get_consts():
    global _CONSTS
    if _CONSTS is None:
        _CONSTS = _consts()
    return _CONSTS


def build(stop_after=None):
    nc = bass.Bass("TRN2", target_bir_lowering=False)

    def din(name, shape, dt=F32):
        return nc.dram_tensor(name, list(shape), dt, kind="ExternalInput").ap()

    x_d = din("x", [L, D])
    pos_d = din("pos", [L, D])
    ctx_d = din("ctx", [LC, D])
    cT_d = din("cT", [128, 16])
    adaw_d = din("ada_w", [D, 6 * D])
    adab_d = din("ada_bT", [128, 48])
    n1g_d = din("n1gT", [128, 8])
    n2g_d = din("n2gT", [128, 8])
    win_d = din("w_in", [D, 3072])
    lbT_d = din("lbT", [128, 16])
    lbrow_d = din("lbrow", [1, 2048])
    gn_d = din("gn", [128, 1])
    fw_d = din("fnet_w", [128, 512])
    wout_d = din("w_out", [D, D])
    rw_d = din("rw", [128, 256])
    rb_d = din("rb", [1, 32])
    w1_d = din("w1r", [NE * 8, 128, 2048])
    b1_d = din("b1T", [128, NE * 16])
    w2_d = din("w2", [NE * 8, 128, D])
    b2_d = din("b2", [NE, D])
    fg_d = din("fg", [1, D])
    ident_d = din("ident", [128, 128])
    ones_d = din("ones", [128, 128])
    cm_d = din("cm", [128, 898])
    dftc_d = din("dftc", [128, 256])
    dftl_d = din("dftl", [16, 128, 4096], BF16)
    out_d = nc.dram_tensor("out", [L, D], F32, kind="ExternalOutput").ap()
    dbg_d = nc.dram_tensor("dbg", [128, 4608], F32, kind="ExternalOutput").ap() if stop_after is not None else None

    with ExitStack() as es:
        def sb(name, shape, dt=F32, stack=es):
            return stack.enter_context(nc.sbuf_tensor(name, list(shape), dt))

        def sem_alloc(name):
            return es.enter_context(nc.semaphore(name))

        P = Prog(nc, sem_alloc)
        PS = [es.enter_context(nc.psum_tensor("ps%d" % i, [128, 512], F32)) for i in range(8)]
        psi = [0]

        def nps():
            i = psi[0] % 8
            psi[0] += 1
            return PS[i], "ps%d" % i

        def dma(q, out, in_, reads, writes, key):
            P.op(q, lambda e: e.dma_start(out=out, in_=in_), reads, writes, semkey=key)

        def mm(out, lhsT, rhs, st, sp, reads, writes):
            P.op("pe", lambda e: e.matmul(out, lhsT, rhs, start=st, stop=sp), reads, writes)

        def tr(out, in_, reads, writes):
            P.op("pe", lambda e: e.transpose(out, in_, IDENT[:]), list(reads) + ["ident"], writes)

        def act(out, in_, func, reads, writes, bias=None, scale=None, accum=None):
            kw = {}
            if bias is not None:
                kw["bias"] = bias
            if scale is not None:
                kw["scale"] = scale
            if accum is not None:
                kw["accum_out"] = accum
            P.op("act", lambda e: e.activation(out, in_, func, **kw), reads, writes)

        def ts(eng, out, in0, s1, s2, op0, op1, reads, writes):
            if op1 is None:
                P.op(eng, lambda e: e.tensor_scalar(out, in0, s1, None, op0), reads, writes)
            else:
                P.op(eng, lambda e: e.tensor_scalar(out, in0, s1, s2, op0, op1), reads, writes)

        def tt(eng, out, in0, in1, op, reads, writes):
            P.op(eng, lambda e: e.tensor_tensor(out, in0, in1, op), reads, writes)

        def stt(eng, out, in0, scalar, in1, op0, op1, reads, writes):
            P.op(eng, lambda e: e.scalar_tensor_tensor(out, in0, scalar, in1, op0, op1), reads, writes)

        def cp(eng, out, in_, reads, writes):
            P.op(eng, lambda e: e.tensor_copy(out, in_), reads, writes)

        def memset(eng, ap, val, writes):
            P.op(eng, lambda e: e.memset(ap, val), (), writes)

        ACC = sb("ACC", [128, NT * D])
        IDENT = sb("IDENT", [128, 128])
        ONES = sb("ONES", [128, 128])
        GATE = sb("GATE", [128, NT * NE])
        COLS = sb("COLS", [128, 64])
        MODT = sb("MODT", [128, 96])
        MX5BC = sb("MX5BC", [128, D])
        EPSC = sb("EPSC", [128, 1])

        def acck(t):
            return "acc%d" % t

        A1X, A1C, A2C, LBT, OMLT, GNC = 0, 8, 16, 24, 32, 40

        def col(base, i):
            return COLS[:, base + i:base + i + 1]

        def modcol(j, which):
            return MODT[:, j * 2 + which:j * 2 + which + 1]


        with ExitStack() as s1:
            XNT = sb("XNT", [128, 8 * 2304], BF16, stack=s1)
            MX2BC = sb("MX2BC", [128, D], stack=s1)
            CM = sb("CM", [128, 898], stack=s1)
            LBBC = sb("LBBC", [128, 1024], stack=s1)
            OMLBC = sb("OMLBC", [128, 1024], stack=s1)

            def xk(t):
                return "xnt%d" % t

            def xnt(kc, t0, n):
                return XNT[:, kc * 2304 + t0:kc * 2304 + t0 + n]

            with ExitStack() as sa:
                G1 = "G:p1"
                G0 = "G:p0"
                CT = sb("CT", [128, 16], stack=sa)
                SC = sb("SC", [128, 16], stack=sa)
                ADAB = sb("ADAB", [128, 48], stack=sa)
                N1G = sb("N1G", [128, 8], stack=sa)
                N2G = sb("N2G", [128, 8], stack=sa)
                LBTR = sb("LBTR", [128, 16], stack=sa)
                TMPC = sb("TMPC", [128, 16], stack=sa)
                DG = [sb("DG%d" % i, [128, 128], stack=sa) for i in range(2)]
                AW = [sb("AW%d" % i, [128, 8 * 512], stack=sa) for i in range(2)]
                dma("sp", IDENT[:], ident_d, (), ["ident"], G0)
                dma("sp", ONES[:], ones_d, (), ["ones"], G0)
                dma("sp", CT[:], cT_d, (), ["ct"], G0)
                dma("sp", ADAB[:], adab_d, (), ["adab"], G0)
                dma("sp", N1G[:], n1g_d, (), ["n1g"], G0)
                dma("sp", N2G[:], n2g_d, (), ["n2g"], G0)
                dma("sp", LBTR[:], lbT_d, (), ["lbtr"], G0)
                dma("sp", COLS[:, GNC:GNC + 1], gn_d, (), ["gnc"], G0)
                memset("dve", EPSC[:], EPS, ["epsc"])
                act(SC[:], CT[:], AF.Silu, ["ct"], ["sc"])
                adaw_v = adaw_d.rearrange("(kc p) n -> p kc n", p=128)
                pm, pmk = nps()
                for j in range(12):
                    s = j % 2
                    dma("sp", AW[s][:].rearrange("p (kc n) -> p kc n", kc=8), adaw_v[:, :, j * 512:(j + 1) * 512],
                        (), ["aw%d" % s], "aw%d" % s)
                    for oc in range(4):
                        gi = j * 4 + oc
                        for kc in range(8):
                            mm(pm[:, gi * 2:gi * 2 + 2], AW[s][:, kc * 512 + oc * 128:kc * 512 + (oc + 1) * 128],
                               SC[:, kc * 2:kc * 2 + 2], kc == 0, kc == 7, ["aw%d" % s, "sc"], [pmk])
                pm3 = pm[:, 0:96].rearrange("p (c t) -> p c t", t=2)
                md3 = MODT[:, 0:96].rearrange("p (c t) -> p c t", t=2)
                for w in range(2):
                    tt("dve", md3[:, :, w], pm3[:, :, w], ADAB[:], ALU.add, [pmk, "adab"], ["modt"])
                mdx = MODT[:, 0:96].rearrange("p (c t) -> p c t", t=2)
                for (base, j0, w, g, gk) in ((A1X, 8, 0, N1G, "n1g"), (A1C, 8, 1, N1G, "n1g"), (A2C, 32, 0, N2G, "n2g")):
                    ts("dve", TMPC[:, 0:8], mdx[:, j0:j0 + 8, w], 1.0, None, ALU.add, None, ["modt"], ["tmpc"])
                    tt("dve", COLS[:, base:base + 8], TMPC[:, 0:8], g[:], ALU.mult, ["tmpc", gk], ["cols"])
                tt("dve", TMPC[:, 0:8], LBTR[:, 0:8], LBTR[:, 8:16], ALU.subtract, ["lbtr"], ["tmpc"])
                act(COLS[:, LBT:LBT + 8], TMPC[:, 0:8], AF.Sigmoid, ["tmpc"], ["cols"])
                ts("dve", COLS[:, OMLT:OMLT + 8], COLS[:, LBT:LBT + 8], -1.0, 1.0, ALU.mult, ALU.add, ["cols"], ["cols"])
                for c in range(8):
                    if c % 4 == 0:
                        pb, pbk = nps()
                    dg = DG[c % 2]
                    ts("dve", dg[:], IDENT[:], modcol(40 + c, 0), None, ALU.mult, None, ["ident", "modt"], ["dg%d" % (c % 2)])
                    mm(pb[:, (c % 4) * 128:(c % 4 + 1) * 128], ONES[:], dg[:], True, True, ["ones", "dg%d" % (c % 2)], [pbk])
                    if c % 4 == 3:
                        cp("dve", MX5BC[:, (c - 3) * 128:(c + 1) * 128], pb[:], [pbk], ["mx5bc"])
                LBR = sb("LBR", [128, 2048], stack=sa)
                POS = [sb("POS%d" % i, [128, D], stack=sa) for i in range(2)]
                XC = [sb("XC%d" % i, [128, D], stack=sa) for i in range(2)]
                YT = [sb("YT%d" % i, [128, D], stack=sa) for i in range(2)]
                SQJ = sb("SQJ", [128, D], BF16, stack=sa)
                SS = sb("SS", [128, NTA], stack=sa)
                RSTD = sb("RSTD", [128, NTA], stack=sa)
                DG2 = [sb("DGb%d" % i, [128, 128], stack=sa) for i in range(2)]
                dma("sp", CM[:], cm_d, (), ["cm"], G1)
                dma("sp", LBR[:], lbrow_d.partition_broadcast(128), (), ["lbr"], G1)
                for t in range(NT):
                    dma("actq", ACC[:, t * D:(t + 1) * D], x_d[t * 128:(t + 1) * 128, :], (), [acck(t)], "xin%d" % t)
                    dma("actq", POS[t % 2][:], pos_d[t * 128:(t + 1) * 128, :], (), ["pos%d" % (t % 2)], "pos%d" % (t % 2))
                    tt("pool", ACC[:, t * D:(t + 1) * D], ACC[:, t * D:(t + 1) * D], POS[t % 2][:], ALU.add,
                       [acck(t), "pos%d" % (t % 2)], [acck(t)])
                    act(SQJ[:], ACC[:, t * D:(t + 1) * D], AF.Square, [acck(t)], ["sqj", "ss"], accum=SS[:, t:t + 1])
                for i in range(2):
                    dma("actq", XC[i][:], ctx_d[i * 128:(i + 1) * 128, :], (), ["xc%d" % i], "xc%d" % i)
                    act(SQJ[:], XC[i][:], AF.Square, ["xc%d" % i], ["sqj", "ss"], accum=SS[:, NT + i:NT + i + 1])
                tt("dve", LBBC[:], LBR[:, 0:1024], LBR[:, 1024:2048], ALU.subtract, ["lbr"], ["lbbc"])
                act(LBBC[:], LBBC[:], AF.Sigmoid, ["lbbc"], ["lbbc"])
                ts("dve", OMLBC[:], LBBC[:], -1.0, 1.0, ALU.mult, ALU.add, ["lbbc"], ["omlbc"])
                for c in range(8):
                    if c % 4 == 0:
                        pb, pbk = nps()
                    dg = DG2[c % 2]
                    ts("dve", dg[:], IDENT[:], modcol(16 + c, 0), None, ALU.mult, None, ["ident", "modt"], ["dgb%d" % (c % 2)])
                    mm(pb[:, (c % 4) * 128:(c % 4 + 1) * 128], ONES[:], dg[:], True, True, ["ones", "dgb%d" % (c % 2)], [pbk])
                    if c % 4 == 3:
                        cp("dve", MX2BC[:, (c - 3) * 128:(c + 1) * 128], pb[:], [pbk], ["mx2bc"])
                act(RSTD[:], SS[:], AF.Sqrt, ["ss", "epsc"], ["rstd"], bias=EPSC[:, 0:1], scale=1.0 / D)
                P.op("dve", lambda e: e.reciprocal(RSTD[:], RSTD[:]), ["rstd"], ["rstd"])
                for t in range(NTA):
                    lat = t < NT
                    src = ACC[:, t * D:(t + 1) * D] if lat else XC[t - NT][:]
                    srck = acck(t) if lat else "xc%d" % (t - NT)
                    y = YT[t % 2]
                    yk = "yt%d" % (t % 2)
                    act(y[:], src, AF.Copy, [srck, "rstd"], [yk], scale=RSTD[:, t:t + 1])
                    abase = A1X if lat else A1C
                    w = 0 if lat else 1
                    for half in range(2):
                        pt, ptk = nps()
                        for c4 in range(4):
                            c = half * 4 + c4
                            tr(pt[:, c4 * 128:(c4 + 1) * 128], y[:, c * 128:(c + 1) * 128], [yk], [ptk])
                        for c4 in range(4):
                            c = half * 4 + c4
                            if half == 0:
                                ts("dve", xnt(c, t * 128, 128), pt[:, c4 * 128:(c4 + 1) * 128], col(abase, c), modcol(c, w),
                                   ALU.mult, ALU.add, [ptk, "cols", "modt"], [xk(t)])
                            else:
                                act(xnt(c, t * 128, 128), pt[:, c4 * 128:(c4 + 1) * 128], AF.Identity,
                                    [ptk, "cols", "modt"], [xk(t)], bias=modcol(c, w), scale=col(abase, c))
                P.emit()

            win_v = win_d.rearrange("(kc p) n -> p kc n", p=128)
            wout_v = wout_d.rearrange("(c p) n -> p c n", p=128)

            with ExitStack() as sn:
              if stop_after != "1A":
                G2 = "G:p2"
                WINF = sb("WINF", [128, 8 * 512], BF16, stack=sn)
                FW = sb("FW", [128, 512], stack=sn)
                DFTC = sb("DFTC", [128, 256], stack=sn)
                CSW = sb("CSW", [128, 4 * 256], BF16, stack=sn)
                WOS = [sb("WOS%d" % i, [128, D], stack=sn) for i in range(1)]
                WOUTF = sb("WOUTF", [128, 4 * D], BF16, stack=sn)
                UT = [sb("UT%d" % i, [128, L], BF16, stack=sn) for i in range(1)]
                AB = sb("AB", [128, 16 * 4 * 256], BF16, stack=sn)
                DF = [sb("DF%d" % i, [128, 4096], BF16, stack=sn) for i in range(2)]
                FXT = sb("FXT", [128, 4 * 512], BF16, stack=sn)
                dma("sp", FW[:], fw_d, (), ["fw"], G2)
                dma("sp", DFTC[:], dftc_d, (), ["dftc"], G2)
                dma("poolq", WINF[:].rearrange("p (kc n) -> p kc n", kc=8), win_v[:, :, 2560:3072], (), ["winf"], "winf")
                for g in range(4):
                    dma("sp", WOS[0][:], wout_v[:, 4 + g, :], (), ["wos0"], "wos0")
                    tt("dve", WOUTF[:, g * D:(g + 1) * D], WOS[0][:], MX2BC[:], ALU.mult,
                       ["wos0", "mx2bc"], ["woutf"])
                for g in range(4):
                    pc, pck = nps()
                    mm(pc[:, 0:128], DFTC[:, 0:128], FW[:, g * 128:(g + 1) * 128], True, True, ["dftc", "fw"], [pck])
                    mm(pc[:, 128:256], DFTC[:, 128:256], FW[:, g * 128:(g + 1) * 128], True, True, ["dftc", "fw"], [pck])
                    cp("dve", CSW[:, g * 256:(g + 1) * 256], pc[:, 0:256], [pck], ["csw"])
                for g in range(4):
                    u = UT[0]
                    uk = "ut0"
                    for t4 in range(4):
                        pu, puk = nps()
                        for kc in range(8):
                            mm(pu[:], WINF[:, kc * 512 + g * 128:kc * 512 + (g + 1) * 128], xnt(kc, t4 * 512, 512),
                               kc == 0, kc == 7, ["winf"] + [xk(t4 * 4 + i) for i in range(4)], [puk])
                        act(u[:, t4 * 512:(t4 + 1) * 512], pu[:], AF.Copy, [puk], [uk])
                    for j2 in range(8):
                        pa, pak = nps()
                        for jj in range(2):
                            j = j2 * 2 + jj
                            mm(pa[:, jj * 256:(jj + 1) * 256], u[:, j * 128:(j + 1) * 128], CSW[:, g * 256:(g + 1) * 256],
                               True, True, [uk, "csw"], [pak])
                        for jj in range(2):
                            j = j2 * 2 + jj
                            o_ap = AB[:, (j * 4 + g) * 256:(j * 4 + g + 1) * 256]
                            if j2 % 2 == 0:
                                cp("dve", o_ap, pa[:, jj * 256:(jj + 1) * 256], [pak], ["ab"])
                            else:
                                act(o_ap, pa[:, jj * 256:(jj + 1) * 256], AF.Copy, [pak], ["ab"])
                pcs = 0
                for lt in range(4):
                    banks = [nps() for _ in range(4)]
                    for cs in range(2):
                        for jh in range(2):
                            sl = pcs % 2
                            dma("sp", DF[sl][:], dftl_d[lt * 4 + cs * 2 + jh], (), ["df%d" % sl], "df%d" % sl)
                            pcs += 1
                            for g in range(4):
                                pg, pgk = banks[g]
                                for j8 in range(8):
                                    j = jh * 8 + j8
                                    first = (cs == 0 and jh == 0 and j8 == 0)
                                    last = (cs == 1 and jh == 1 and j8 == 7)
                                    mm(pg[:], AB[:, (j * 4 + g) * 256 + cs * 128:(j * 4 + g) * 256 + (cs + 1) * 128],
                                       DF[sl][:, j8 * 512:(j8 + 1) * 512], first, last, ["ab", "df%d" % sl], [pgk])
                    for g in range(4):
                        pg, pgk = banks[g]
                        if g % 2 == 0:
                            act(FXT[:, g * 512:(g + 1) * 512], pg[:], AF.Copy, [pgk], ["fxt"])
                        else:
                            cp("dve", FXT[:, g * 512:(g + 1) * 512], pg[:], [pgk], ["fxt"])
                    for tk in range(4):
                        t = lt * 4 + tk
                        for dh in range(2):
                            po, pok = nps()
                            for g in range(4):
                                mm(po[:], FXT[:, g * 512 + tk * 128:g * 512 + (tk + 1) * 128],
                                   WOUTF[:, g * D + dh * 512:g * D + (dh + 1) * 512], g == 0, g == 3, ["fxt", "woutf"], [pok])
                            a_ap = ACC[:, t * D + dh * 512:t * D + (dh + 1) * 512]
                            tt("dve", a_ap, po[:], a_ap, ALU.add, [pok, acck(t)], [acck(t)])
                P.emit()


            with ExitStack() as sh:
              if stop_after not in ("1A", "1N"):
                OTACC = sb("OTACC", [128, 2 * L], stack=sh)
                WQ = sb("WQ", [128, 8 * 256], BF16, stack=sh)
                WV = sb("WV", [128, 8 * 256], BF16, stack=sh)
                WG = sb("WG", [128, 8 * 256], BF16, stack=sh)
                WZ = [sb("WZ%d" % i, [128, 8 * 256], BF16, stack=sh) for i in range(2)]
                WOS2 = sb("WOSb", [128, D], stack=sh)
                WOUTH = sb("WOUTH", [128, 2 * D], BF16, stack=sh)

                def pair(name, dt=F32, w=256):
                    return [sb("%s_%d" % (name, i), [128, w], dt, stack=sh) for i in range(2)]
                T1 = pair("T1"); LOGF = pair("LOGF"); KTM = pair("KTM"); T2 = pair("T2"); KT = pair("KT")
                EPI = pair("EPI", F32, 512); EM = pair("EM"); ERD = pair("ERD", F32, 260)
                VBF = [sb("VBF_%d" % i, [128, 256], BF16, stack=sh) for i in range(3)]; QI = pair("QI", BF16); QE = pair("QE", BF16); KI = pair("KI", BF16)
                KP0 = pair("KP0", BF16); KP1 = pair("KP1", BF16); ATM = pair("ATM", BF16)
                S32 = sb("S32", [128, 256], stack=sh)
                SBF = [sb("SBF%d" % i, [128, 256], BF16, stack=sh) for i in range(2)]
                OO = sb("OO", [128, 256], stack=sh)
                SQ = sb("SQ", [128, 256], stack=sh)
                RS = sb("RS", [128, 256], stack=sh)
                SG = sb("SG", [128, 256], stack=sh)
                HXB = sb("HXB", [128, 256], BF16, stack=sh)
                MC = [CM[:, 0:128], CM[:, 384:512]]
                MI = [CM[:, 128:256], CM[:, 512:640]]
                M2 = [CM[:, 256:384], CM[:, 640:768]]
                SEL = CM[:, 768:770]
                MASK = [CM[:, 770:834], CM[:, 834:898]]
                for p in range(2):
                    memset("dve", ATM[p][:], 0.0, ["atm%d" % p])
                    memset("dve", KP0[p][:], 0.0, ["kp%d" % p])
                    memset("dve", KP1[p][:], 0.0, ["kp%d" % p])

                from collections import deque
                freeps = deque(range(8))

                def aps():
                    i = freeps.popleft()
                    return PS[i], "ps%d" % i

                def rps(key):
                    freeps.append(int(key[2:]))

                def com(it):
                    hp, dr, t, p = it["hp"], it["dr"], it["t"], it["p"]
                    return hp, dr, t, p, t < NT, t * 128, [xk(t)], "%d" % p, "vbf%d" % it["p3"], VBF[it["p3"]]

                def F1a(it):
                    hp, dr, t, p, lat, tok0, xr, P_, vk, vb = com(it)
                    wz, wzk = WZ[dr], "wz%d" % dr
                    lbo = dr * 512 + hp * 256
                    pzv, pzvk = aps()
                    for kc in range(8):
                        mm(pzv[:, 0:256], xnt(kc, tok0, 128), wz[:, kc * 256:(kc + 1) * 256], kc == 0, kc == 7,
                           xr + [wzk], [pzvk])
                    for kc in range(8):
                        mm(pzv[:, 256:512], xnt(kc, tok0, 128), WV[:, kc * 256:(kc + 1) * 256], kc == 0, kc == 7,
                           xr + ["wv"], [pzvk])
                    act(T1[p][:], pzv[:, 0:256], AF.Sigmoid, [pzvk], ["t1" + P_])
                    act(vb[:], pzv[:, 256:512], AF.Copy, [pzvk], [vk])
                    rps(pzvk)
                    tt("dve", T1[p][:], T1[p][:], OMLBC[:, lbo:lbo + 256], ALU.mult, ["t1" + P_, "omlbc"], ["t1" + P_])
                    tt("dve", T1[p][:], T1[p][:], LBBC[:, lbo:lbo + 256], ALU.add, ["t1" + P_, "lbbc"], ["t1" + P_])
                    act(LOGF[p][:], T1[p][:], AF.Ln, ["t1" + P_], ["logf" + P_])
                    ts("dve", KTM[p][:], T1[p][:], -1.0, 1.0, ALU.mult, ALU.add, ["t1" + P_], ["ktm" + P_])

                def F1b(it):
                    hp, dr, t, p, lat, tok0, xr, P_, vk, vb = com(it)
                    if not lat:
                        return
                    pzq, pzqk = aps()
                    for i in range(2):
                        tr(pzq[:, i * 128:(i + 1) * 128], KTM[p][:, i * 128:(i + 1) * 128], ["ktm" + P_], [pzqk])
                    for i in range(2):
                        for kc in range(8):
                            mm(pzq[:, 256 + i * 128:256 + (i + 1) * 128], WQ[:, kc * 256 + i * 128:kc * 256 + (i + 1) * 128],
                               xnt(kc, tok0, 128), kc == 0, kc == 7, xr + ["wq"], [pzqk])
                    it["pzq"] = (pzq, pzqk)

                def F2a(it):
                    hp, dr, t, p, lat, tok0, xr, P_, vk, vb = com(it)
                    pr, prk = aps()
                    mm(pr[:, 0:256], M2[dr], LOGF[p][:], True, True, ["cm", "logf" + P_], [prk])
                    for i in range(2):
                        mm(pr[:, 256 + i * 2:256 + i * 2 + 2], LOGF[p][:, i * 128:(i + 1) * 128], SEL, True, True,
                           ["cm", "logf" + P_], [prk])
                    if lat:
                        pcm, pcmk = aps()
                        for i in range(2):
                            mm(pcm[:, i * 128:(i + 1) * 128], LOGF[p][:, i * 128:(i + 1) * 128], MC[dr], True, True,
                               ["cm", "logf" + P_], [pcmk])
                        for i in range(2):
                            mm(pcm[:, 256 + i * 128:256 + (i + 1) * 128], LOGF[p][:, i * 128:(i + 1) * 128], MI[dr], True, True,
                               ["cm", "logf" + P_], [pcmk])
                    act(ERD[p][:], pr[:, 0:260], AF.Exp, [prk], ["er" + P_, "dec" + P_])
                    rps(prk)
                    if lat:
                        act(EPI[p][:], pcm[:, 0:512], AF.Exp, [pcmk], ["ep" + P_, "ei" + P_])
                        rps(pcmk)
                        P.op("dve", lambda e: e.reciprocal(EM[p][:], EPI[p][:, 0:256]), ["ep" + P_], ["em" + P_])

                def F2b(it):
                    hp, dr, t, p, lat, tok0, xr, P_, vk, vb = com(it)
                    tt("dve", KP0[p][0:64, :], KTM[p][0:64, :], ERD[p][0:64, 0:256], ALU.mult, ["ktm" + P_, "er" + P_], ["kp" + P_])
                    tt("dve", KP1[p][64:128, :], KTM[p][64:128, :], ERD[p][64:128, 0:256], ALU.mult, ["ktm" + P_, "er" + P_], ["kp" + P_])
                    pu, puk = aps()
                    for c in range(2):
                        kp = KP0[p] if c == 0 else KP1[p]
                        for i in range(2):
                            mm(pu[:, c * 256 + i * 128:c * 256 + (i + 1) * 128], kp[:, i * 128:(i + 1) * 128],
                               vb[:, i * 128:(i + 1) * 128], True, True, ["kp" + P_, vk], [puk])
                    it["pu"] = (pu, puk)
                    if not lat:
                        return
                    pzq, pzqk = it["pzq"]
                    tt("dve", QI[p][:], pzq[:, 256:512], EPI[p][:, 0:256], ALU.mult, [pzqk, "ep" + P_], ["qi" + P_])
                    tt("dve", KI[p][:], pzq[:, 0:256], EM[p][:], ALU.mult, [pzqk, "em" + P_], ["ki" + P_])
                    tt("dve", QE[p][:], pzq[:, 256:512], EPI[p][:, 256:512], ALU.mult, [pzqk, "ei" + P_], ["qe" + P_])
                    rps(pzqk)
                    pa, pak = aps()
                    for i in range(2):
                        mm(pa[:, i * 128:(i + 1) * 128], KI[p][:, i * 128:(i + 1) * 128], QI[p][:, i * 128:(i + 1) * 128],
                           True, True, ["ki" + P_, "qi" + P_], [pak])
                    for i in range(2):
                        for c in range(2):
                            r0, r1 = c * 64, (c + 1) * 64
                            tt("dve", ATM[p][r0:r1, i * 128 + c * 64:i * 128 + (c + 1) * 64],
                               pa[r0:r1, i * 128 + c * 64:i * 128 + (c + 1) * 64], MASK[dr][r0:r1, :], ALU.mult,
                               [pak, "cm"], ["atm" + P_])
                    rps(pak)

                def chunk_step(it, ci):
                    hp, dr, t, p, lat, tok0, xr, P_, vk, vb = com(it)
                    pu, puk = it["pu"]
                    c = ((0, 1) if dr == 0 else (1, 0))[ci]
                    sidx = it["sidx"]
                    if lat:
                        for i in range(2):
                            po, pok = it["pos"][i]
                            mm(po[:, c * 64:(c + 1) * 64], SBF[sidx][:, i * 128:(i + 1) * 128],
                               QE[p][:, i * 128 + c * 64:i * 128 + (c + 1) * 64], False, ci == 1,
                               ["sbf%d" % sidx, "qe" + P_], [pok])
                    for i in range(2):
                        stt("dve", S32[:, i * 128:(i + 1) * 128], S32[:, i * 128:(i + 1) * 128],
                            ERD[p][:, 256 + i * 2 + c:256 + i * 2 + c + 1], pu[:, c * 256 + i * 128:c * 256 + (i + 1) * 128],
                            ALU.mult, ALU.add, ["s32", "dec" + P_, puk], ["s32"])
                    nidx = 1 - sidx
                    act(SBF[nidx][:], S32[:], AF.Copy, ["s32"], ["sbf%d" % nidx])
                    it["sidx"] = nidx

                def Ba(it):
                    hp, dr, t, p, lat, tok0, xr, P_, vk, vb = com(it)
                    it["sidx"] = 0
                    if lat:
                        it["pos"] = [aps() for _ in range(2)]
                        for i in range(2):
                            po, pok = it["pos"][i]
                            mm(po[:, 0:128], vb[:, i * 128:(i + 1) * 128], ATM[p][:, i * 128:(i + 1) * 128], True, False,
                               [vk, "atm" + P_], [pok])
                    chunk_step(it, 0)

                def Bb(it):
                    hp, dr, t, p, lat, tok0, xr, P_, vk, vb = com(it)
                    chunk_step(it, 1)
                    rps(it["pu"][1])
                    if not lat:
                        return
                    if dr == 0:
                        for i in range(2):
                            po, pok = it["pos"][i]
                            cp("dve", OTACC[:, i * L + tok0:i * L + tok0 + 128], po[:, 0:128], [pok], ["otacc%d" % t])
                            rps(pok)
                        return
                    for i in range(2):
                        po, pok = it["pos"][i]
                        tt("dve", OO[:, i * 128:(i + 1) * 128], po[:, 0:128], OTACC[:, i * L + tok0:i * L + tok0 + 128],
                           ALU.add, [pok, "otacc%d" % t], ["oo"])
                        rps(pok)
                    act(SQ[:], OO[:], AF.Square, ["oo"], ["sq"])

                def Bc(it):
                    hp, dr, t, p, lat, tok0, xr, P_, vk, vb = com(it)
                    if not lat or dr == 0:
                        return
                    pss, pssk = aps()
                    for i in range(2):
                        for kc in range(8):
                            mm(pss[:, 256 + i * 128:256 + (i + 1) * 128], WG[:, kc * 256 + i * 128:kc * 256 + (i + 1) * 128],
                               xnt(kc, tok0, 128), kc == 0, kc == 7, xr + ["wg"], [pssk])
                    mm(pss[:, 0:256], ONES[:], SQ[:], True, True, ["ones", "sq"], [pssk])
                    act(RS[:], pss[:, 0:256], AF.Sqrt, [pssk, "epsc"], ["rs"], bias=EPSC[:, 0:1], scale=1.0 / 128)
                    act(SG[:], pss[:, 256:512], AF.Sigmoid, [pssk], ["sg"])
                    rps(pssk)
                    P.op("dve", lambda e: e.reciprocal(RS[:], RS[:]), ["rs"], ["rs"])
                    tt("dve", OO[:], OO[:], RS[:], ALU.mult, ["oo", "rs"], ["oo"])
                    stt("dve", HXB[:], OO[:], COLS[:, GNC:GNC + 1], SG[:], ALU.mult, ALU.mult, ["oo", "gnc", "sg"], ["hxb"])
                    for dh in range(2):
                        px, pxk = aps()
                        for i in range(2):
                            mm(px[:], HXB[:, i * 128:(i + 1) * 128], WOUTH[:, i * D + dh * 512:i * D + (dh + 1) * 512],
                               i == 0, i == 1, ["hxb", "wouth"], [pxk])
                        a_ap = ACC[:, t * D + dh * 512:t * D + (dh + 1) * 512]
                        tt("dve", a_ap, px[:], a_ap, ALU.add, [pxk, acck(t)], [acck(t)])
                        rps(pxk)

                itn = 0
                for hp in range(2):
                    c0 = hp * 256

                    def wload(dst, colbase, key):
                        dma("poolq", dst[:].rearrange("p (kc n) -> p kc n", kc=8), win_v[:, :, colbase + c0:colbase + c0 + 256],
                            (), [key], key)
                    wload(WZ[0], 1536, "wz0")
                    wload(WV, 512, "wv")
                    wload(WQ, 0, "wq")
                    wload(WZ[1], 2048, "wz1")
                    wload(WG, 1024, "wg")
                    for i in range(2):
                        dma("sp", WOS2[:], wout_v[:, hp * 2 + i, :], (), ["wosb"], "wosb")
                        tt("pool", WOUTH[:, i * D:(i + 1) * D], WOS2[:], MX2BC[:], ALU.mult, ["wosb", "mx2bc"], ["wouth"])
                    for dr in range(2):
                        memset("dve", S32[:], 0.0, ["s32"])
                        memset("dve", SBF[0][:], 0.0, ["sbf0"])
                        order = [16, 17] + list(range(16)) if dr == 0 else [17, 16] + list(range(15, -1, -1))
                        its = []
                        for t in order:
                            its.append({"hp": hp, "dr": dr, "t": t, "p": itn % 2, "p3": itn % 3, "cur": 0})
                            itn += 1
                        ni = len(its)
                        F1a(its[0]); F1b(its[0])
                        F1a(its[1]); F1b(its[1])
                        F2a(its[0]); F2b(its[0])
                        for n in range(ni):
                            n1 = its[n + 1] if n + 1 < ni else None
                            n2 = its[n + 2] if n + 2 < ni else None
                            if n1: F2a(n1)
                            Ba(its[n])
                            if n2: F1a(n2)
                            if n1: F2b(n1)
                            Bb(its[n])
                            if n2: F1b(n2)
                            Bc(its[n])
                P.emit()

        with ExitStack() as s2:
            HMT = sb("HMT", [128, 8 * L], BF16, stack=s2)
            SS2 = sb("SS2", [128, NT], stack=s2)
            RSTD2 = sb("RSTD2", [128, NT], stack=s2)
            B1T = sb("B1T", [128, NE * 16], stack=s2)

            def hk(t):
                return "hmt%d" % t

            if stop_after is None or stop_after in ("2", "3"):
                with ExitStack() as sr:
                    G3 = "G:p3"
                    RW = sb("RW", [128, 256], stack=sr)
                    RBBC = sb("RBBC", [128, NE], stack=sr)
                    B2P = sb("B2P", [NE, D], stack=sr)
                    SQJ2 = sb("SQJ2", [128, D], BF16, stack=sr)
                    Y2 = [sb("Y2_%d" % i, [128, D], stack=sr) for i in range(2)]
                    HM32 = [sb("HM32_%d" % i, [128, D], stack=sr) for i in range(2)]
                    LG = sb("LG", [128, NE], stack=sr)
                    M8 = sb("M8", [128, 8], stack=sr)
                    NEGM = sb("NEGM", [128, 1], stack=sr)
                    EX = sb("EX", [128, NE], stack=sr)
                    MSK = sb("MSK", [128, NE], stack=sr)
                    SUMC = sb("SUMC", [128, 1], stack=sr)
                    GT = [sb("GT%d" % i, [NE, 128], stack=sr) for i in range(2)]
                    dma("sp", RW[:], rw_d, (), ["rw"], G3)
                    dma("sp", RBBC[:], rb_d.partition_broadcast(128), (), ["rbbc"], G3)
                    dma("sp", B2P[:], b2_d, (), ["b2p"], G3)
                    dma("sp", B1T[:], b1_d, (), ["b1t"], G3)
                    tt("dve", B2P[:], B2P[:], MX5BC[0:NE, :], ALU.mult, ["b2p", "mx5bc"], ["b2p"])
                    b13 = B1T[:].rearrange("p (e c) -> p e c", c=16)
                    ts("dve", b13[:, :, 8:16], b13[:, :, 8:16], 1.0, None, ALU.add, None, ["b1t"], ["b1t"])
                    for t in range(NT):
                        act(SQJ2[:], ACC[:, t * D:(t + 1) * D], AF.Square, [acck(t)], ["sqj2", "ss2"], accum=SS2[:, t:t + 1])
                    act(RSTD2[:], SS2[:], AF.Sqrt, ["ss2", "epsc"], ["rstd2"], bias=EPSC[:, 0:1], scale=1.0 / D)
                    P.op("dve", lambda e: e.reciprocal(RSTD2[:], RSTD2[:]), ["rstd2"], ["rstd2"])
                    def stA(t):
                        y = Y2[t % 2]
                        yk = "y2_%d" % (t % 2)
                        h32 = HM32[t % 2]
                        h32k = "hm32_%d" % (t % 2)
                        act(y[:], ACC[:, t * D:(t + 1) * D], AF.Copy, [acck(t), "rstd2"], [yk], scale=RSTD2[:, t:t + 1])
                        for half in range(2):
                            pt, ptk = nps()
                            for c4 in range(4):
                                c = half * 4 + c4
                                tr(pt[:, c4 * 128:(c4 + 1) * 128], y[:, c * 128:(c + 1) * 128], [yk], [ptk])
                            for c4 in range(4):
                                c = half * 4 + c4
                                if half == 0:
                                    ts("dve", h32[:, c * 128:(c + 1) * 128], pt[:, c4 * 128:(c4 + 1) * 128], col(A2C, c),
                                       modcol(24 + c, 0), ALU.mult, ALU.add, [ptk, "cols", "modt"], [h32k])
                                else:
                                    act(h32[:, c * 128:(c + 1) * 128], pt[:, c4 * 128:(c4 + 1) * 128], AF.Identity,
                                        [ptk, "cols", "modt"], [h32k], bias=modcol(24 + c, 0), scale=col(A2C, c))
                        for c in range(8):
                            eng = "pool" if c % 2 == 0 else "act"
                            if eng == "pool":
                                cp("pool", HMT[:, c * L + t * 128:c * L + (t + 1) * 128], h32[:, c * 128:(c + 1) * 128], [h32k], [hk(t)])
                            else:
                                act(HMT[:, c * L + t * 128:c * L + (t + 1) * 128], h32[:, c * 128:(c + 1) * 128], AF.Copy, [h32k], [hk(t)])
                        pl, plk = nps()
                        for c in range(8):
                            mm(pl[:, 0:NE], h32[:, c * 128:(c + 1) * 128], RW[:, c * NE:(c + 1) * NE], c == 0, c == 7,
                               [h32k, "rw"], [plk])
                        return (h32, h32k, pl, plk)
                    def stB(t, st):
                        h32, h32k, pl, plk = st
                        tt("dve", LG[:], pl[:, 0:NE], RBBC[:], ALU.add, [plk, "rbbc"], ["lg"])
                        P.op("dve", lambda e: e.max(out=M8[:], in_=LG[:]), ["lg"], ["m8"])
                        ts("dve", NEGM[:], M8[:, 0:1], -1.0, None, ALU.mult, None, ["m8"], ["negm"])
                        act(EX[:], LG[:], AF.Exp, ["lg", "negm"], ["ex"], bias=NEGM[:, 0:1], scale=1.0)
                        ts("dve", MSK[:], LG[:], M8[:, 3:4], None, ALU.is_ge, None, ["lg", "m8"], ["msk"])
                        tt("dve", EX[:], EX[:], MSK[:], ALU.mult, ["ex", "msk"], ["ex"])
                        P.op("dve", lambda e: e.tensor_reduce(SUMC[:], EX[:], AX.X, ALU.add), ["ex"], ["sumc"])
                        P.op("dve", lambda e: e.reciprocal(SUMC[:], SUMC[:]), ["sumc"], ["sumc"])
                        g_ap = GATE[:, t * NE:(t + 1) * NE]
                        ts("dve", g_ap, EX[:], SUMC[:, 0:1], None, ALU.mult, None, ["ex", "sumc"], ["gate%d" % t])
                        pgt, pgtk = nps()
                        tr(pgt[0:NE, 0:128], g_ap, ["gate%d" % t], [pgtk])
                        gt = GT[t % 2]
                        gtk = "gt%d" % (t % 2)
                        act(gt[:], pgt[0:NE, 0:128], AF.Copy, [pgtk], [gtk])
                        for dh in range(2):
                            pb2, pb2k = nps()
                            mm(pb2[:], gt[:], B2P[:, dh * 512:(dh + 1) * 512], True, True, [gtk, "b2p"], [pb2k])
                            a_ap = ACC[:, t * D + dh * 512:t * D + (dh + 1) * 512]
                            tt("dve", a_ap, pb2[:], a_ap, ALU.add, [pb2k, acck(t)], [acck(t)])
                    stq = {0: stA(0)}
                    for t in range(NT):
                        if t + 1 < NT:
                            stq[t + 1] = stA(t + 1)
                        stB(t, stq.pop(t))
                    P.emit()

            if stop_after is None or stop_after == "3":
                with ExitStack() as sm:
                    HHT = sb("HHT", [128, 8 * L], BF16, stack=sm)
                    W1S = [sb("W1S%d" % i, [128, 2048], stack=sm) for i in range(2)]
                    W1B = [sb("W1B%d" % i, [128, 2048], BF16, stack=sm) for i in range(2)]
                    W2S = [sb("W2S%d" % i, [128, D], stack=sm) for i in range(2)]
                    W2B = sb("W2B", [128, 8 * D], BF16, stack=sm)
                    AG = [sb("AG%d" % i, [128, 512], stack=sm) for i in range(2)]
                    SGM = [sb("SGM%d" % i, [128, 512], stack=sm) for i in range(2)]
                    TL = [sb("TL%d" % i, [128, 512], stack=sm) for i in range(2)]
                    QQ = [sb("QQ%d" % i, [128, 512], stack=sm) for i in range(2)]
                    allh = [hk(t) for t in range(NT)]
                    npc = NE * 8

                    def issue_loads(n):
                        e, fc = divmod(n, 8)
                        s = n % 2
                        dma("sp", W1S[s][:], w1_d[n], (), ["w1s%d" % s], "w1s%d" % s)
                        dma("sp", W2S[s][:], w2_d[n], (), ["w2s%d" % s], "w2s%d" % s)

                    def cast_w1(m):
                        sm = m % 2
                        act(W1B[sm][:], W1S[sm][:], AF.Copy, ["w1s%d" % sm], ["w1b%d" % sm])

                    issue_loads(0)
                    cast_w1(0)
                    ucnt = 0
                    for n in range(npc):
                        e, fc = divmod(n, 8)
                        s = n % 2
                        if n + 1 < npc:
                            issue_loads(n + 1)
                        tt("pool", W2B[:, fc * D:(fc + 1) * D], W2S[s][:], MX5BC[:], ALU.mult,
                           ["w2s%d" % s, "mx5bc"], ["w2b%d" % fc])
                        for t4 in range(4):
                            u = ucnt % 2
                            ucnt += 1
                            hr = [hk(t4 * 4 + i) for i in range(4)]
                            pgl = []
                            for half in range(2):
                                pb, pbk = nps()
                                pgl.append((pb, pbk))
                                for kc in range(8):
                                    mm(pb[:], W1B[s][:, kc * 256 + half * 128:kc * 256 + (half + 1) * 128],
                                       HMT[:, kc * L + t4 * 512:kc * L + (t4 + 1) * 512], kc == 0, kc == 7,
                                       ["w1b%d" % s] + hr, [pbk])
                            (pgp, pgk), (plp, plk) = pgl
                            b1g = B1T[:, e * 16 + fc:e * 16 + fc + 1]
                            b1l = B1T[:, e * 16 + 8 + fc:e * 16 + 8 + fc + 1]
                            ts("dve", AG[u][:], pgp[:], b1g, 7.0, ALU.add, ALU.min, [pgk, "b1t"], ["ag%d" % u])
                            act(SGM[u][:], AG[u][:], AF.Sigmoid, ["ag%d" % u], ["sgm%d" % u], scale=1.702)
                            ts("dve", TL[u][:], plp[:], b1l, 8.0, ALU.add, ALU.min, [plk, "b1t"], ["tl%d" % u])
                            stt("dve", QQ[u][:], TL[u][:], -6.0, AG[u][:], ALU.max, ALU.mult, ["tl%d" % u, "ag%d" % u], ["qq%d" % u])
                            tt("pool", HHT[:, fc * L + t4 * 512:fc * L + (t4 + 1) * 512], QQ[u][:], SGM[u][:], ALU.mult,
                               ["qq%d" % u, "sgm%d" % u], ["hht%d_%d" % (fc, t4)])
                            if t4 == 0 and n + 1 < npc:
                                cast_w1(n + 1)
                        if fc == 7:
                            for tk in range(NT):
                                for dh in range(2):
                                    po, pok = nps()
                                    for f in range(8):
                                        mm(po[:], HHT[:, f * L + tk * 128:f * L + (tk + 1) * 128],
                                           W2B[:, f * D + dh * 512:f * D + (dh + 1) * 512], f == 0, f == 7,
                                           ["hht%d_%d" % (f, tk // 4), "w2b%d" % f], [pok])
                                    a_ap = ACC[:, tk * D + dh * 512:tk * D + (dh + 1) * 512]
                                    stt("dve", a_ap, po[:], GATE[:, tk * NE + e:tk * NE + e + 1], a_ap, ALU.mult, ALU.add,
                                        [pok, "gate%d" % tk, acck(tk)], [acck(tk)])
                    P.emit()

            with ExitStack() as sf:
                G4 = "G:p4"
                FGBC = sb("FGBC", [128, D], stack=sf)
                SQJ3 = sb("SQJ3", [128, D], BF16, stack=sf)
                SS3 = sb("SS3", [128, NT], stack=sf)
                OUTT = [sb("OUTT%d" % i, [128, D], stack=sf) for i in range(2)]
                dma("sp", FGBC[:], fg_d.partition_broadcast(128), (), ["fgbc"], G4)
                final_norm = stop_after is None
                if final_norm:
                    for t in range(NT):
                        act(SQJ3[:], ACC[:, t * D:(t + 1) * D], AF.Square, [acck(t)], ["sqj3", "ss3"], accum=SS3[:, t:t + 1])
                    act(SS3[:], SS3[:], AF.Sqrt, ["ss3", "epsc"], ["ss3"], bias=EPSC[:, 0:1], scale=1.0 / D)
                    P.op("dve", lambda e: e.reciprocal(SS3[:], SS3[:]), ["ss3"], ["ss3"])
                for t in range(NT):
                    o = OUTT[t % 2]
                    ok = "outt%d" % (t % 2)
                    if final_norm:
                        stt("dve", o[:], ACC[:, t * D:(t + 1) * D], SS3[:, t:t + 1], FGBC[:], ALU.mult, ALU.mult,
                            [acck(t), "ss3", "fgbc"], [ok])
                    else:
                        cp("dve", o[:], ACC[:, t * D:(t + 1) * D], [acck(t)], [ok])
                    dma("sp", out_d[t * 128:(t + 1) * 128, :], o[:], [ok], (), "out%d" % (t % 2))
                P.out_keys = ["out0", "out1"]
                P.emit(final_wait_out=True)
    return nc


def _prep_inputs(x, c, ctx, c_ctx, ada_w, ada_b, norm1_g, w_in, hgrn_lb, hgrn_gnorm, fnet_w, w_out,
                 norm2_g, router_w, router_b, moe_w1, moe_b1, moe_w2, moe_b2, final_g):
    f = lambda a: np.ascontiguousarray(np.asarray(a, dtype=np.float32))
    cs = _get_consts()
    shared = {}
    shared["pos"] = cs["pos"]
    shared["ada_w"] = f(ada_w[0])
    shared["ada_bT"] = f(np.asarray(ada_b[0]).reshape(48, 128).T)
    shared["n1gT"] = f(np.asarray(norm1_g[0]).reshape(8, 128).T)
    shared["n2gT"] = f(np.asarray(norm2_g[0]).reshape(8, 128).T)
    shared["w_in"] = f(w_in[0])
    lb = np.asarray(hgrn_lb, dtype=np.float32)
    shared["lbT"] = f(lb.reshape(2, 2, 4, 128).transpose(3, 0, 1, 2).reshape(128, 16))
    shared["lbrow"] = f(lb.reshape(1, 2048))
    shared["gn"] = f(np.asarray(hgrn_gnorm[0]).reshape(128, 1))
    shared["fnet_w"] = f(np.asarray(fnet_w[0]).transpose(1, 0, 2).reshape(128, 512))
    shared["w_out"] = f(w_out[0])
    shared["rw"] = f(np.asarray(router_w[0]).reshape(8, 128, NE).transpose(1, 0, 2).reshape(128, 256))
    shared["rb"] = f(np.asarray(router_b[0]).reshape(1, NE))
    w1 = np.asarray(moe_w1[0], dtype=np.float32)
    w1r = w1.reshape(NE, 8, 128, 2, 8, 128).transpose(0, 4, 2, 1, 3, 5)
    shared["w1r"] = np.ascontiguousarray(w1r).reshape(NE * 8, 128, 2048)
    shared["b1T"] = f(np.asarray(moe_b1[0]).reshape(NE, 16, 128).transpose(2, 0, 1).reshape(128, NE * 16))
    shared["w2"] = f(moe_w2[0]).reshape(NE * 8, 128, D)
    shared["b2"] = f(moe_b2[0])
    shared["fg"] = f(np.asarray(final_g).reshape(1, D))
    shared["ident"] = cs["ident"]
    shared["ones"] = cs["ones"]
    shared["cm"] = cs["cm"]
    shared["dftc"] = cs["dftc"]
    shared["dftl"] = cs["dftl"]
    x = np.asarray(x, dtype=np.float32)
    c = np.asarray(c, dtype=np.float32)
    ctx = np.asarray(ctx, dtype=np.float32)
    c_ctx = np.asarray(c_ctx, dtype=np.float32)
    in_maps = []
    for b in range(8):
        m = dict(shared)
        m["x"] = np.ascontiguousarray(x[b])
        m["ctx"] = np.ascontiguousarray(ctx[b])
        cc = np.stack([c[b].reshape(8, 128).T, c_ctx.reshape(8, 128).T], axis=-1)
        m["cT"] = np.ascontiguousarray(cc.reshape(128, 16)).astype(np.float32)
        in_maps.append(m)
    return in_maps


_NC_CACHE = {}


def kernel(**inputs):
    in_maps = _prep_inputs(**inputs)
    if "nc" not in _NC_CACHE:
        _NC_CACHE["nc"] = build()
    nc = _NC_CACHE["nc"]
    res = run_bass_kernel_spmd(nc, in_maps, core_ids=list(range(8)))
    out = np.stack([np.asarray(r["out"], dtype=np.float32) for r in res.results], axis=0)
    return out.reshape(8, L, D)
```
